# Optimizing a Trainium2 kernel written in Bass

```python
import jax, jax.numpy as jnp
from jax import lax
import numpy as np

D_MODEL = 1024
BATCH = 8
SEQ = 8192
DEPTH = 2

CHUNK = 64
D_MIX = D_MODEL
EPS = 1e-6
NEG = -1e30

ATT_HEAD_DIM = 64
D_ATT = D_MIX // 4
ATT_HEADS = D_ATT // ATT_HEAD_DIM
ATT_LEFT_CHUNKS = 8
ATT_BAND = (ATT_LEFT_CHUNKS + 1) * CHUNK
MAX_REL = 128
N_REL = 2 * MAX_REL + 1

D_ML = D_MIX // 2
ML_HEADS = 4
ML_HEAD_DIM = D_ML // ML_HEADS
ML_CONV = 4

D_CONV = D_MIX - D_ATT - D_ML
CONV_WIDTH = 31

IN_SIZES = (D_ATT, D_ATT, D_ATT, D_ATT,
            D_ML, D_ML, D_ML, D_ML, D_ML,
            ML_HEADS, ML_HEADS,
            D_CONV, D_CONV, D_CONV)
D_IN = 4 * D_ATT + 5 * D_ML + 2 * ML_HEADS + 3 * D_CONV

kernel_name = "hybrid_chunk_attn_mlstm_conformer_conv"


def split_columns(h):
    parts = []
    off = 0
    for s in IN_SIZES:
        parts.append(h[..., off:off + s])
        off += s
    return parts


def rms_norm(x, g):
    xf = x.astype(jnp.float32)
    y = xf * lax.rsqrt(jnp.mean(xf * xf, axis=-1, keepdims=True) + EPS)
    return (y * g.astype(jnp.float32)).astype(x.dtype)


def layer_norm(x, g, b):
    xf = x.astype(jnp.float32)
    mu = jnp.mean(xf, axis=-1, keepdims=True)
    xc = xf - mu
    y = xc * lax.rsqrt(jnp.mean(xc * xc, axis=-1, keepdims=True) + EPS)
    return (y * g.astype(jnp.float32) + b.astype(jnp.float32)).astype(x.dtype)


def causal_depthwise_conv(x, w, b):
    width, ch = w.shape
    xp = jnp.pad(x, ((0, 0), (width - 1, 0), (0, 0)))
    out = lax.conv_general_dilated(
        xp, w[:, None, :].astype(x.dtype), window_strides=(1,), padding='VALID',
        dimension_numbers=('NWC', 'WIO', 'NWC'), feature_group_count=ch)
    return out + b.astype(x.dtype)


def chunk_band_attention(q, k, v, q_g, k_g, rel_bias):
    B, S, H, Dh = q.shape
    n_chunks = S // CHUNK
    pad = ATT_LEFT_CHUNKS * CHUNK
    q = rms_norm(q, q_g)
    k = rms_norm(k, k_g)
    kp = jnp.pad(k, ((0, 0), (pad, 0), (0, 0), (0, 0)))
    vp = jnp.pad(v, ((0, 0), (pad, 0), (0, 0), (0, 0)))
    rel = jnp.arange(CHUNK)[:, None] - jnp.arange(ATT_BAND)[None, :] + pad
    rel_idx = jnp.clip(rel, -MAX_REL, MAX_REL) + MAX_REL
    bias = rel_bias[:, rel_idx].astype(jnp.float32)
    scale = Dh ** -0.5
    qc = q.reshape(B, n_chunks, CHUNK, H, Dh).transpose(1, 0, 3, 2, 4)

    def one_chunk(args):
        c, qb = args
        start = c * CHUNK
        kb = lax.dynamic_slice_in_dim(kp, start, ATT_BAND, axis=1)
        vb = lax.dynamic_slice_in_dim(vp, start, ATT_BAND, axis=1)
        s = jnp.einsum('bhqd,bkhd->bhqk', qb, kb,
                       preferred_element_type=jnp.float32) * scale + bias
        key_pos = start - pad + jnp.arange(ATT_BAND)
        s = jnp.where(key_pos[None, None, None, :] >= 0, s, NEG)
        p = jax.nn.softmax(s, axis=-1).astype(vb.dtype)
        return jnp.einsum('bhqk,bkhd->bqhd', p, vb)

    out = lax.map(one_chunk, (jnp.arange(n_chunks), qc))
    return out.transpose(1, 0, 2, 3, 4).reshape(B, S, H * Dh)


def mlstm_chunkwise(q, k, v, i_pre, f_pre):
    B, S, H, D = q.shape
    n_chunks = S // CHUNK
    f32 = jnp.float32

    def chunks4(a):
        return a.astype(f32).reshape(B, n_chunks, CHUNK, H, D).transpose(1, 0, 3, 2, 4)

    def chunks3(a):
        return a.astype(f32).reshape(B, n_chunks, CHUNK, H).transpose(1, 0, 3, 2)

    qc = chunks4(q)
    kc = chunks4(k) * (D ** -0.5)
    vc = chunks4(v)
    ic = chunks3(i_pre)
    lfc = chunks3(jax.nn.log_sigmoid(f_pre.astype(f32)))
    causal = jnp.tril(jnp.ones((CHUNK, CHUNK), dtype=bool))

    def step(carry, xs):
        C, n, m = carry
        qb, kb, vb, ib, lfb = xs
        b = jnp.cumsum(lfb, axis=-1)
        log_d = b[..., :, None] - b[..., None, :] + ib[..., None, :]
        log_d = jnp.where(causal, log_d, NEG)
        inter = b + m[..., None]
        m_t = jnp.maximum(jnp.max(log_d, axis=-1), inter)
        d_mat = jnp.exp(log_d - m_t[..., None])
        inter_w = jnp.exp(inter - m_t)
        s = jnp.einsum('bhtd,bhsd->bhts', qb, kb) * d_mat
        num = (jnp.einsum('bhts,bhsd->bhtd', s, vb)
               + inter_w[..., None] * jnp.einsum('bhvk,bhtk->bhtv', C, qb))
        den = jnp.sum(s, axis=-1) + inter_w * jnp.einsum('bhk,bhtk->bht', n, qb)
        h = num / jnp.maximum(jnp.abs(den), jnp.exp(-m_t))[..., None]
        b_last = b[..., -1]
        w_log = b_last[..., None] - b + ib
        m_new = jnp.maximum(b_last + m, jnp.max(w_log, axis=-1))
        decay = jnp.exp(b_last + m - m_new)
        w = jnp.exp(w_log - m_new[..., None])
        C_new = decay[..., None, None] * C + jnp.einsum('bhs,bhsv,bhsk->bhvk', w, vb, kb)
        n_new = decay[..., None] * n + jnp.einsum('bhs,bhsk->bhk', w, kb)
        return (C_new, n_new, m_new), h

    init = (jnp.zeros((B, H, D, D), f32), jnp.zeros((B, H, D), f32), jnp.zeros((B, H), f32))
    _, hs = lax.scan(step, init, (qc, kc, vc, ic, lfc))
    return hs.transpose(1, 0, 3, 2, 4).reshape(B, S, H, D)


def setup_inputs(seed: int = 0) -> dict:
    key = jax.random.key(seed)
    ks = jax.random.split(key, 17)
    f32 = jnp.float32
    nrm = lambda k, shape: jax.random.normal(k, shape, f32)
    return {
        "x": nrm(ks[0], (BATCH, SEQ, D_MODEL)),
        "norm_g": 1.0 + 0.02 * nrm(ks[1], (DEPTH, D_MODEL)),
        "w_in": nrm(ks[2], (DEPTH, D_MODEL, D_IN)) * D_MODEL ** -0.5,
        "att_q_g": 1.0 + 0.02 * nrm(ks[3], (DEPTH, ATT_HEAD_DIM)),
        "att_k_g": 1.0 + 0.02 * nrm(ks[4], (DEPTH, ATT_HEAD_DIM)),
        "att_rel_bias": 0.1 * nrm(ks[5], (DEPTH, ATT_HEADS, N_REL)),
        "ml_qk_conv_w": nrm(ks[6], (DEPTH, ML_CONV, 2 * D_ML)) * ML_CONV ** -0.5,
        "ml_qk_conv_b": 0.02 * nrm(ks[7], (DEPTH, 2 * D_ML)),
        "ml_b_i": 0.1 * nrm(ks[8], (DEPTH, ML_HEADS)),
        "ml_b_f": jnp.linspace(3.0, 6.0, ML_HEADS, dtype=f32)[None, :]
                  + 0.1 * nrm(ks[9], (DEPTH, ML_HEADS)),
        "ml_out_g": 1.0 + 0.02 * nrm(ks[10], (DEPTH, D_ML)),
        "cv_dw_w": nrm(ks[11], (DEPTH, CONV_WIDTH, D_CONV)) * CONV_WIDTH ** -0.5,
        "cv_dw_b": 0.02 * nrm(ks[12], (DEPTH, D_CONV)),
        "cv_ln_g": 1.0 + 0.02 * nrm(ks[13], (DEPTH, D_CONV)),
        "cv_ln_b": 0.02 * nrm(ks[14], (DEPTH, D_CONV)),
        "w_out": nrm(ks[15], (DEPTH, D_MIX, D_MODEL)) * D_MIX ** -0.5,
    }


def reference(x, norm_g, w_in, att_q_g, att_k_g, att_rel_bias, ml_qk_conv_w, ml_qk_conv_b,
              ml_b_i, ml_b_f, ml_out_g, cv_dw_w, cv_dw_b, cv_ln_g, cv_ln_b, w_out):
    B, S, _ = x.shape
    for l in range(DEPTH):
        h = rms_norm(x, norm_g[l])
        proj = jnp.einsum('bsd,de->bse', h, w_in[l])
        (a_q, a_k, a_v, a_z, m_q, m_k, m_v, m_o, m_z, m_i, m_f,
         c_a, c_b, c_z) = split_columns(proj)

        att = chunk_band_attention(
            a_q.reshape(B, S, ATT_HEADS, ATT_HEAD_DIM),
            a_k.reshape(B, S, ATT_HEADS, ATT_HEAD_DIM),
            a_v.reshape(B, S, ATT_HEADS, ATT_HEAD_DIM),
            att_q_g[l], att_k_g[l], att_rel_bias[l])
        att = att * jax.nn.silu(a_z)

        qk = jax.nn.silu(causal_depthwise_conv(jnp.concatenate([m_q, m_k], axis=-1),
                                               ml_qk_conv_w[l], ml_qk_conv_b[l]))
        m_q, m_k = qk[..., :D_ML], qk[..., D_ML:]
        i_pre = m_i.astype(jnp.float32) + ml_b_i[l].astype(jnp.float32)
        f_pre = m_f.astype(jnp.float32) + ml_b_f[l].astype(jnp.float32)
        hm = mlstm_chunkwise(
            m_q.reshape(B, S, ML_HEADS, ML_HEAD_DIM),
            m_k.reshape(B, S, ML_HEADS, ML_HEAD_DIM),
            m_v.reshape(B, S, ML_HEADS, ML_HEAD_DIM), i_pre, f_pre)
        hm = jax.nn.sigmoid(m_o.astype(jnp.float32)).reshape(B, S, ML_HEADS, ML_HEAD_DIM) * hm
        hm = rms_norm(hm, ml_out_g[l].reshape(ML_HEADS, ML_HEAD_DIM).astype(jnp.float32))
        ml = hm.reshape(B, S, D_ML).astype(x.dtype) * jax.nn.silu(m_z)

        u = c_a * jax.nn.sigmoid(c_b)
        u = causal_depthwise_conv(u, cv_dw_w[l], cv_dw_b[l])
        u = jax.nn.silu(layer_norm(u, cv_ln_g[l], cv_ln_b[l]))
        cv = u * jax.nn.silu(c_z)

        mixed = jnp.concatenate([att.astype(x.dtype), ml, cv.astype(x.dtype)], axis=-1)
        x = x + jnp.einsum('bse,ed->bsd', mixed, w_out[l])
    return x
```

```python
import math
import numpy as np
import concourse.bass as bass
import concourse.mybir as mybir
from concourse.bass_utils import run_bass_kernel_spmd

F32 = mybir.dt.float32
BF16 = mybir.dt.bfloat16
AF = mybir.ActivationFunctionType
ALU = mybir.AluOpType
AX = mybir.AxisListType

D = 1024
DIN = 4360
EPS = 1e-6
NEGB = -30000.0
P_GIN, P_QG, P_KG, P_MLG, P_LNG, P_LNB, P_DWB, P_BIF, P_QKCW, P_QKCB, P_DWW = (
    0, 8, 72, 136, 648, 904, 1160, 1416, 1424, 1456, 1464)
NPRM = 1526


class Res:
    __slots__ = ("name", "w", "r", "excl")

    def __init__(self, name, excl=False):
        self.name = name
        self.w = None
        self.r = {}
        self.excl = excl


class Sched:
    ENGS = ("pe", "act", "dve", "pool", "sp")

    def __init__(self):
        self.streams = {e: [] for e in self.ENGS}
        self.cnt = {e: 0 for e in self.ENGS}
        self.known = {e: {} for e in self.ENGS}
        self.dma_cnt = {}

    def _deps(self, eng, reads, writes):
        deps = {}

        def add(k, v):
            if deps.get(k, 0) < v:
                deps[k] = v
        for r in reads:
            if r.w is not None:
                add(*r.w)
            if r.excl:
                for k, v in r.r.items():
                    if k != eng:
                        add(k, v)
        for w in writes:
            if w.w is not None:
                add(*w.w)
            for k, v in w.r.items():
                if k != eng:
                    add(k, v)
        waits = []
        for k, v in deps.items():
            if k == eng and eng == "pe":
                continue
            if self.known[eng].get(k, 0) >= v:
                continue
            self.known[eng][k] = v
            waits.append((k, v))
        return waits

    @staticmethod
    def _mark(tok, reads, writes):
        k, v = tok
        for r in reads:
            if r.r.get(k, 0) < v:
                r.r[k] = v
        for w in writes:
            w.w = tok
            w.r = {}

    def op(self, eng, fn, reads=(), writes=()):
        waits = self._deps(eng, reads, writes)
        self.cnt[eng] += 1
        tok = (eng, self.cnt[eng])
        self._mark(tok, reads, writes)
        self.streams[eng].append([waits, fn, tok, None])

    def dma(self, q, sem, fn, reads=(), writes=()):
        waits = self._deps(q, reads, writes)
        self.dma_cnt[sem] = self.dma_cnt.get(sem, 0) + 16
        tok = (sem, self.dma_cnt[sem])
        self._mark(tok, reads, writes)
        self.streams[q].append([waits, fn, None, (sem, 16)])

    def emit(self, nc, block, final_waits):
        waited = {e: set() for e in self.ENGS}
        for e in self.ENGS:
            for waits, fn, tok, dm in self.streams[e]:
                for k, v in waits:
                    if k in waited:
                        waited[k].add(v)
        rank = {}
        for e in self.ENGS:
            srt = sorted(waited[e])
            rank[e] = {v: i + 1 for i, v in enumerate(srt)}
        sem_names = list(self.ENGS) + sorted(self.dma_cnt.keys())
        sems = {}
        ctxs = []
        for n in sem_names:
            c = nc.semaphore("s_" + n)
            sems[n] = c.__enter__()
            ctxs.append(c)

        def run(e, eng):
            for waits, fn, tok, dm in self.streams[e]:
                for k, v in waits:
                    if k in rank:
                        eng.wait_ge(sems[k], rank[k][v])
                    else:
                        eng.wait_ge(sems[k], v)
                ins = fn(eng)
                if dm is not None:
                    ins.then_inc(sems[dm[0]], dm[1])
                elif tok[1] in rank[e]:
                    ins.then_inc(sems[e], 1)
            if e == "sp":
                for k, v in final_waits:
                    eng.wait_ge(sems[k], v)

        @block.tensor
        def _(eng):
            run("pe", eng)

        @block.scalar
        def _(eng):
            run("act", eng)

        @block.vector
        def _(eng):
            run("dve", eng)

        @block.gpsimd
        def _(eng):
            run("pool", eng)

        @block.sync
        def _(eng):
            run("sp", eng)
        return ctxs


def build_nc(S, L):
    NT = S // 128
    nc = bass.Bass("TRN2", target_bir_lowering=False)
    x_d = nc.dram_tensor("x", [S, D], F32, kind="ExternalInput").ap()
    win_d = nc.dram_tensor("w_in", [L, D, DIN], F32, kind="ExternalInput").ap()
    wout_d = nc.dram_tensor("w_out", [L, D, D], F32, kind="ExternalInput").ap()
    prm_d = nc.dram_tensor("prm", [L, 128, NPRM], F32, kind="ExternalInput").ap()
    attb_d = nc.dram_tensor("attb", [L, 128, 2560], F32, kind="ExternalInput").ap()
    cst_d = nc.dram_tensor("cst", [128, 896], F32, kind="ExternalInput").ap()
    rowp_d = nc.dram_tensor("rowp", [L, 1, 1024], F32, kind="ExternalInput").ap()
    out_d = nc.dram_tensor("out", [S, D], F32, kind="ExternalOutput").ap()
    mid_d = nc.dram_tensor("xmid", [S, D], F32, kind="Internal").ap() if L > 1 else None

    sch = Sched()
    stack = []

    def sb(name, shape, dt):
        c = nc.sbuf_tensor(name, shape, dt)
        t = c.__enter__()
        stack.append(c)
        return t

    def ps(name, shape, dt):
        c = nc.psum_tensor(name, shape, dt)
        t = c.__enter__()
        stack.append(c)
        return t

    w_in = sb("w_in_sb", [128, 8, DIN], BF16)
    w_out = sb("w_out_sb", [128, 8, D], BF16)
    prm = sb("prm_sb", [128, NPRM], F32)
    cst = sb("cst_sb", [128, 896], F32)
    identb = sb("identb", [128, 128], BF16)
    attb = sb("attb_sb", [128, 20, 128], BF16)
    dgc = sb("dgc", [128, 62, 128], BF16)
    dgq = sb("dgq", [128, 32, 128], BF16)
    negqb = sb("negqb", [128, 8], F32)
    xt = [sb("xt%d" % i, [128, D], F32) for i in range(4)]
    stg = [xt[2], xt[3]]
    browb = sb("browb", [1, 1024], BF16)
    onesb = sb("onesb", [1, 128], BF16)
    st1 = sb("st1", [128, 8], F32)
    hb = [sb("hb0", [128, D], BF16)] * 2
    hT = [sb("hT%d" % i, [128, 8, 128], BF16) for i in range(2)]
    qk32 = sb("qk32", [128, 512], F32)
    sq = sb("sq", [128, 512], BF16)
    sq2 = sb("sq2", [128, 512], BF16)
    st8 = sb("st8", [128, 8], F32)
    qkn = sb("qkn", [128, 512], BF16)
    qT = [sb("qT%d" % i, [128, 2, 128], BF16) for i in range(2)]
    kTr = [sb("kTr%d" % i, [128, 2, 128], BF16) for i in range(6)]
    vr = [sb("vr%d" % i, [128, 4, 65], BF16) for i in range(6)]
    zs = [sb("zs%d" % i, [128, 256], BF16) for i in range(2)]
    tz = sb("tz", [128, 256], F32)
    PT = [sb("PT%d" % i, [128, 5, 128], BF16) for i in range(2)]
    rs4 = sb("rs4", [128, 4], F32)
    mixed = [sb("mixed%d" % i, [128, D], BF16) for i in range(2)]
    mT = sb("mT", [128, 8, 128], BF16)
    vaug = sb("vaug", [128, 4, 129], F32)
    vt = [sb("vt%d" % i, [128, 4, 129], BF16) for i in range(2)]
    vh = [sb("vh%d" % i, [128, 4, 129], BF16) for i in range(2)]
    so = [sb("so%d" % i, [128, 512], F32) for i in range(2)]
    mz = [sb("mz%d" % i, [128, 512], F32) for i in range(2)]
    ifs = sb("ifs", [128, 8], F32)
    nl = sb("nl", [128, 4], F32)
    gt = sb("gt", [128, 4], F32)
    g4 = sb("g4", [128, 4], F32)
    thr = [sb("thr%d" % i, [128, 4], F32) for i in range(2)]
    dec = [sb("dec%d" % i, [128, 4], F32) for i in range(2)]
    den = sb("den", [128, 4], F32)
    rden = sb("rden", [128, 4], F32)
    qkraw = sb("qkraw", [128, 8, 131], BF16)
    tq = sb("tq", [128, 512], F32)
    qkT = [sb("qkT%d" % i, [128, 8, 128], BF16) for i in range(2)]
    ktm = [sb("ktm%d" % i, [128, 512], BF16) for i in range(2)]
    PTm = [sb("PTm%d" % i, [128, 4, 128], BF16) for i in range(2)]
    Cst = sb("Cst", [128, 4, 129], F32)
    Cb = sb("Cb", [128, 4, 129], BF16)
    hm = sb("hm", [128, 512], F32)
    st4 = sb("st4", [128, 4], F32)
    uT = [sb("uT%d" % i, [128, 2, 158], BF16) for i in range(2)]
    uc = sb("uc", [128, 256], F32)
    bst = sb("bst", [128, 6], F32)
    mv = sb("mv", [128, 2], F32)
    rstd1 = sb("rstd1", [128, 1], F32)
    yln = sb("yln", [128, 256], F32)
    czs = [sb("czs%d" % i, [128, 256], BF16) for i in range(2)]
    tcz = sb("tcz", [128, 256], F32)
    tcz2 = tcz
    tyl2 = sb("tyl2", [128, 256], F32)
    T0 = ps("T0", [128, 8, 128], BF16)
    FB = [ps("F%d" % i, [128, 512], F32) for i in range(7)]

    R = {}

    def res(name, excl=False):
        if name not in R:
            R[name] = Res(name, excl)
        return R[name]
    rT0 = res("T0", True)
    rF = [res("F%d" % i, True) for i in range(7)]
    rot = [0]

    def rotbank():
        i = 1 + rot[0] % 5
        rot[0] += 1
        return FB[i], rF[i]

    def act(fn, reads, writes):
        sch.op("act", fn, reads, writes)

    def dve(fn, reads, writes):
        sch.op("dve", fn, reads, writes)

    def pool(fn, reads, writes):
        sch.op("pool", fn, reads, writes)

    def pe(fn, reads, writes):
        sch.op("pe", fn, reads, writes)

    def sigmoid_chain(src_ap, src_res, tmp_ap, tmp_res, bias_neg=None):
        if bias_neg is None:
            act(lambda e: e.activation(out=tmp_ap, in_=src_ap, func=AF.Exp, scale=-1.0), [src_res], [tmp_res])
        else:
            act(lambda e: e.activation(out=tmp_ap, in_=src_ap, func=AF.Exp, scale=-1.0, bias=bias_neg),
                [src_res], [tmp_res])
        act(lambda e: e.activation(out=tmp_ap, in_=tmp_ap, func=AF.Ln, bias=1.0), [tmp_res], [tmp_res])
        act(lambda e: e.activation(out=tmp_ap, in_=tmp_ap, func=AF.Exp, scale=-1.0), [tmp_res], [tmp_res])

    def rstd_chain(ap, r, scale):
        act(lambda e: e.activation(out=ap, in_=ap, func=AF.Ln, scale=scale, bias=EPS), [r], [r])
        act(lambda e: e.activation(out=ap, in_=ap, func=AF.Exp, scale=-0.5), [r], [r])

    r_cst, r_identb = res("cst"), res("identb")
    sch.dma("sp", "d_cst", lambda e: e.dma_start(out=cst[:, :], in_=cst_d[:, :]), [], [r_cst])
    dve(lambda e: e.tensor_copy(out=identb[:, :], in_=cst[:, 0:128]), [r_cst], [r_identb])
    ident32 = cst[:, 0:128]
    tri = cst[:, 128:256]
    mask4 = cst[:, 256:768]
    ones128 = cst[:, 768:896]

    r_win, r_wout, r_prm, r_attb = res("w_in"), res("w_out"), res("prm"), res("attb")
    r_stg = [res("xt2"), res("xt3")]
    r_dgc, r_dgq, r_negqb = res("dgc"), res("dgq"), res("negqb")
    r_x = [res("xt0"), res("xt1"), res("xt2"), res("xt3")]
    r_mid = [res("mid%d" % t) for t in range(NT)]
    r_kT = [res("kT%d" % i) for i in range(6)]
    r_v = [res("v%d" % i) for i in range(6)]
    names = ["qT0", "qT1", "zs0", "zs1", "uT0", "uT1", "czs0", "czs1", "browb", "onesb", "mixed0", "mixed1", "vt0", "vt1", "vh0", "vh1", "so0", "so1", "mz0", "mz1", "thr0", "thr1",
             "dec0", "dec1", "qkT0", "qkT1", "ktm0", "ktm1", "PTm0", "PTm1", "st1", "hb0", "hT0", "hT1", "qk32", "sq", "sq2", "st8", "qkn", "qT", "zs", "tz", "PT0", "PT1", "rs4", "mixed", "mT",
             "vaug", "vt", "vh", "so", "mz", "ifs", "nl", "gt", "g4", "thr", "dec", "den", "rden",
             "qkraw", "tq", "qkT", "ktm", "PTm", "Cst", "Cb", "hm", "st4", "uT", "tcb", "uc", "bst",
             "mv", "rstd1", "yln", "tyl2", "tcz"]
    r_ = {n: res(n) for n in names}
    dve(lambda e: e.tensor_copy(out=onesb[:, :], in_=cst[0:1, 768:896]), [r_cst], [r_["onesb"]])
    stg_i = [0]

    pool(lambda e: e.memset(vaug[:, :, :], 1.0), [], [r_["vaug"]])
    for i in range(6):
        pool(lambda e, i=i: e.memset(vr[i][:, :, :], 1.0), [], [r_v[i]])

    final_waits = []

    for l in range(L):
        src_d = x_d if l == 0 else mid_d
        dst_d = out_d if l == L - 1 else mid_d
        sch.dma("sp", "d_prm", lambda e, l=l: e.dma_start(out=prm[:, :], in_=prm_d[l, :, :]), [], [r_prm])
        def stage(src_ap, ncols, cast_fn):
            s = stg_i[0] % 2
            stg_i[0] += 1
            sch.dma("sp", "d_stg%d" % s, lambda e, s=s: e.dma_start(out=stg[s][:, 0:ncols], in_=src_ap),
                    [], [r_stg[s]])
            cast_fn(stg[s][:, 0:ncols], r_stg[s], stg_i[0] % 2 == 0)

        s0 = stg_i[0] % 2
        stg_i[0] += 1
        sch.dma("sp", "d_stg%d" % s0, lambda e, l=l, s0=s0: e.dma_start(out=stg[s0][0:1, :], in_=rowp_d[l, :, :]),
                [], [r_stg[s0]])
        dve(lambda e, s0=s0: e.tensor_copy(out=browb[:, :], in_=stg[s0][0:1, :]), [r_stg[s0]], [r_["browb"]])
        for c0, n in ((0, 1024), (1024, 1024), (2048, 512)):
            def cast_b(src, rs, use_act, c0=c0, n=n):
                dve(lambda e: e.tensor_copy(out=attb[:, c0 // 128:(c0 + n) // 128, :],
                                            in_=src.rearrange("p (a b) -> p a b", b=128)), [rs], [r_attb])
            stage(attb_d[l, :, c0:c0 + n], n, cast_b)
        for k in range(8):
            for c0, n in ((0, 1024), (1024, 1024), (2048, 1024), (3072, 1024), (4096, 264)):
                def cast_w(src, rs, use_act, k=k, c0=c0, n=n):
                    if use_act:
                        act(lambda e: e.activation(out=w_in[:, k, c0:c0 + n], in_=src, func=AF.Copy,
                                                   scale=prm[:, P_GIN + k:P_GIN + k + 1]), [rs, r_prm], [r_win])
                    else:
                        dve(lambda e: e.tensor_scalar(out=w_in[:, k, c0:c0 + n], in0=src,
                                                      scalar1=prm[:, P_GIN + k:P_GIN + k + 1], scalar2=None,
                                                      op0=ALU.mult), [rs, r_prm], [r_win])
                stage(win_d[l, k * 128:(k + 1) * 128, c0:c0 + n], n, cast_w)
        for k in range(8):
            def cast_o(src, rs, use_act, k=k):
                if use_act:
                    act(lambda e: e.activation(out=w_out[:, k, :], in_=src, func=AF.Copy), [rs], [r_wout])
                else:
                    dve(lambda e: e.tensor_copy(out=w_out[:, k, :], in_=src), [rs], [r_wout])
            stage(wout_d[l, k * 128:(k + 1) * 128, :], 1024, cast_o)
        for i in range(62):
            pool(lambda e, i=i: e.tensor_scalar(out=dgc[:, i, :], in0=ident32, scalar1=prm[:, P_DWW + i:P_DWW + i + 1],
                                                scalar2=None, op0=ALU.mult),
                 [r_cst, r_prm], [r_dgc])
        for i in range(32):
            pool(lambda e, i=i: e.tensor_scalar(out=dgq[:, i, :], in0=ident32,
                                                scalar1=prm[:, P_QKCW + i:P_QKCW + i + 1],
                                                scalar2=None, op0=ALU.mult),
                 [r_cst, r_prm], [r_dgq])
        dve(lambda e: e.tensor_scalar(out=negqb[:, :], in0=prm[:, P_QKCB:P_QKCB + 8], scalar1=-1.0, scalar2=None,
                                      op0=ALU.mult), [r_prm], [r_negqb])
        pool(lambda e: e.memset(Cst[:, :, :], 0.0), [], [r_["Cst"]])
        pool(lambda e: e.memset(Cb[:, :, :], 0.0), [], [r_["Cb"]])
        pool(lambda e: e.memset(uT[1][:, :, 128:158], 0.0), [], [r_["uT1"]])
        pool(lambda e: e.memset(qkraw[:, :, 0:3], 0.0), [], [r_["qkraw"]])

        def load(t, l=l, src_d=src_d):
            b = t % 4
            rd = [r_mid[t]] if l > 0 else []
            sch.dma("sp", "d_x%d" % b,
                    lambda e: e.dma_start(out=xt[b][:, :], in_=src_d[t * 128:(t + 1) * 128, :]), rd, [r_x[b]])

        def proj_tm(bank, rb, c0, n, hTc, rhT):
            for k in range(8):
                pe(lambda e, k=k: e.matmul(bank[:, 0:n], lhsT=hTc[:, k, :], rhs=w_in[:, k, c0:c0 + n],
                                           start=(k == 0), stop=(k == 7)), [rhT, r_win], [rb])

        def proj_fm(bank, rb, c0, ntile, hTc, rhT):
            for j in range(ntile):
                for k in range(8):
                    pe(lambda e, k=k, j=j: e.matmul(bank[:, j * 128:(j + 1) * 128],
                                                    lhsT=w_in[:, k, c0 + j * 128:c0 + (j + 1) * 128],
                                                    rhs=hTc[:, k, :], start=(k == 0), stop=(k == 7)),
                       [rhT, r_win], [rb])

        def front(t):
            c = t % 2
            X, rX = xt[t % 4], r_x[t % 4]
            hbc, hTc = hb[c], hT[c]
            rhb, rhT = r_["hb0"], r_["hT%d" % c]
            dve(lambda e: e.scalar_tensor_tensor(out=hbc[:, :], in0=X[:, :], scalar=1.0, in1=X[:, :],
                                                 op0=ALU.mult, op1=ALU.mult, accum_out=st1[:, 0:1]),
                [rX], [rhb, r_["st1"]])
            rstd_chain(st1[:, 0:1], r_["st1"], 1.0 / D)
            dve(lambda e: e.tensor_scalar(out=hbc[:, :], in0=X[:, :], scalar1=st1[:, 0:1], scalar2=None,
                                          op0=ALU.mult), [rX, r_["st1"]], [rhb])
            yield
            for k in range(8):
                pe(lambda e, k=k: e.transpose(out=T0[:, k, :], in_=hbc[:, k * 128:(k + 1) * 128], identity=identb[:, :]),
                   [rhb, r_identb], [rT0])
            act(lambda e: e.activation(out=hTc[:, 0:4, :], in_=T0[:, 0:4, :], func=AF.Copy), [rT0], [rhT])
            dve(lambda e: e.tensor_copy(out=hTc[:, 4:8, :], in_=T0[:, 4:8, :]), [rT0], [rhT])
            yield

        def attA(t):
            c = t % 2
            hTc, rhT = hT[c], r_["hT%d" % c]
            slot = t % 6
            zsc, rzs = zs[c], r_["zs%d" % c]
            qTc, rqT = qT[c], r_["qT%d" % c]
            bank, rb = rotbank()
            proj_tm(bank, rb, 0, 512, hTc, rhT)
            act(lambda e, bank=bank: e.activation(out=qk32[:, :], in_=bank[:, :], func=AF.Copy), [rb], [r_["qk32"]])
            yield
            dve(lambda e: e.tensor_tensor(out=sq[:, :], in0=qk32[:, :], in1=qk32[:, :], op=ALU.mult),
                [r_["qk32"]], [r_["sq"]])
            dve(lambda e: e.tensor_reduce(out=st8[:, :], in_=sq[:, :].rearrange("p (g d) -> p g d", d=64),
                                          axis=AX.X, op=ALU.add), [r_["sq"]], [r_["st8"]])
            rstd_chain(st8[:, :], r_["st8"], 1.0 / 64)
            dve(lambda e: e.tensor_scalar(out=st8[:, 0:4], in0=st8[:, 0:4], scalar1=0.125, scalar2=None, op0=ALU.mult),
                [r_["st8"]], [r_["st8"]])
            yield
            bank, rb = rotbank()
            proj_tm(bank, rb, 512, 512, hTc, rhT)
            act(lambda e, bank=bank: e.activation(
                out=vr[slot][:, :, 0:64], in_=bank[:, 0:256].rearrange("p (h d) -> p h d", d=64), func=AF.Copy),
                [rb], [r_v[slot]])
            sigmoid_chain(bank[:, 256:512], rb, tz[:, 0:256], r_["tz"])
            dve(lambda e, bank=bank: e.tensor_tensor(out=zsc[:, :], in0=bank[:, 256:512], in1=tz[:, 0:256], op=ALU.mult),
                [rb, r_["tz"]], [rzs])
            yield
            for g in range(8):
                gcol = P_QG if g < 4 else P_KG
                dve(lambda e, g=g, gcol=gcol: e.scalar_tensor_tensor(
                    out=qkn[:, g * 64:(g + 1) * 64], in0=qk32[:, g * 64:(g + 1) * 64], scalar=st8[:, g:g + 1],
                    in1=prm[:, gcol:gcol + 64], op0=ALU.mult, op1=ALU.mult),
                    [r_["qk32"], r_["st8"], r_prm], [r_["qkn"]])
            yield
            for i in range(4):
                pe(lambda e, i=i: e.transpose(out=T0[:, i, :], in_=qkn[:, i * 128:(i + 1) * 128], identity=identb[:, :]),
                   [r_["qkn"], r_identb], [rT0])
            act(lambda e: e.activation(out=qTc[:, :, :], in_=T0[:, 0:2, :], func=AF.Copy), [rT0], [rqT])
            dve(lambda e: e.tensor_copy(out=kTr[slot][:, :, :], in_=T0[:, 2:4, :]), [rT0], [r_kT[slot]])
            yield

        def attB(t):
            c = t % 2
            zsc, rzs = zs[c], r_["zs%d" % c]
            qTc, rqT = qT[c], r_["qT%d" % c]
            jmin = max(0, 4 - t)

            def scores(h):
                p, half = h // 2, h % 2
                lo = 64 * half
                PTc, rPT = PT[h % 2], r_["PT%d" % (h % 2)]
                bx, rbx = rotbank()
                by, rby = rotbank()
                for j in range(jmin, 5):
                    ks = (t - 4 + j) % 6
                    if j < 4:
                        o_ap, ro = bx[:, j * 128:(j + 1) * 128], rbx
                    else:
                        o_ap, ro = by[:, 0:128], rby
                    pe(lambda e, o_ap=o_ap, ks=ks: e.matmul(
                        o_ap, lhsT=kTr[ks][lo:lo + 64, p, :], rhs=qTc[lo:lo + 64, p, :], start=True, stop=False),
                       [r_kT[ks], rqT], [ro])
                    pe(lambda e, o_ap=o_ap, j=j: e.matmul(
                        o_ap, lhsT=identb[:, :], rhs=attb[:, h * 5 + j, :], start=False, stop=True),
                       [r_identb, r_attb], [ro])
                if jmin < 4:
                    act(lambda e: e.activation(
                        out=PTc[:, jmin:4, :], in_=bx[:, jmin * 128:512].rearrange("p (j q) -> p j q", q=128),
                        func=AF.Exp), [rbx], [rPT])
                act(lambda e: e.activation(out=PTc[:, 4, :], in_=by[:, 0:128], func=AF.Exp), [rby], [rPT])

            def pv(h):
                PTc, rPT = PT[h % 2], r_["PT%d" % (h % 2)]
                for j in range(jmin, 5):
                    ks = (t - 4 + j) % 6
                    pe(lambda e, ks=ks, j=j: e.matmul(
                        FB[6][:, h * 65:(h + 1) * 65], lhsT=PTc[:, j, :], rhs=vr[ks][:, h, :],
                        start=(j == jmin), stop=(j == 4)),
                       [rPT, r_v[ks]], [rF[6]])

            scores(0)
            yield
            for h in range(4):
                if h + 1 < 4:
                    scores(h + 1)
                    yield
                pv(h)
                yield
            dve(lambda e: e.reciprocal(out=rs4[:, :],
                                       in_=FB[6][:, 0:260].rearrange("p (h d) -> p h d", d=65)[:, :, 64]),
                [rF[6]], [r_["rs4"]])
            for h in range(4):
                dve(lambda e, h=h: e.scalar_tensor_tensor(
                    out=mixed[c][:, h * 64:(h + 1) * 64], in0=FB[6][:, h * 65:h * 65 + 64],
                    scalar=rs4[:, h:h + 1], in1=zsc[:, h * 64:(h + 1) * 64], op0=ALU.mult, op1=ALU.mult),
                    [rF[6], r_["rs4"], rzs], [r_["mixed%d" % c]])
            yield

        def mlsA(t):
            c = t % 2
            hTc, rhT = hT[c], r_["hT%d" % c]
            vtc, vhc, soc, mzc, thrc, decc = vt[c], vh[c], so[c], mz[c], thr[c], dec[c]
            qkTc, ktmc, PTmc = qkT[c], ktm[c], PTm[c]
            rvt, rvh, rso, rmz, rthr, rdec = (r_["vt%d" % c], r_["vh%d" % c], r_["so%d" % c], r_["mz%d" % c],
                                               r_["thr%d" % c], r_["dec%d" % c])
            rqkT, rktm, rPTm = r_["qkT%d" % c], r_["ktm%d" % c], r_["PTm%d" % c]
            bk, rbk = rotbank()
            proj_tm(bk, rbk, 2048, 512, hTc, rhT)
            act(lambda e, bk=bk: e.activation(out=vaug[:, :, 0:128],
                                       in_=bk[:, :].rearrange("p (h d) -> p h d", d=128), func=AF.Copy),
                [rbk], [r_["vaug"]])
            yield
            bk, rbk = rotbank()
            proj_tm(bk, rbk, 3584, 8, hTc, rhT)
            dve(lambda e, bk=bk: e.tensor_tensor(out=ifs[:, :], in0=bk[:, 0:8], in1=prm[:, P_BIF:P_BIF + 8],
                                          op=ALU.add), [rbk, r_prm], [r_["ifs"]])
            act(lambda e: e.activation(out=nl[:, :], in_=ifs[:, 4:8], func=AF.Exp, scale=-1.0), [r_["ifs"]], [r_["nl"]])
            act(lambda e: e.activation(out=nl[:, :], in_=nl[:, :], func=AF.Ln, bias=1.0), [r_["nl"]], [r_["nl"]])
            yield
            bk, rbk = rotbank()
            pe(lambda e, bk=bk: e.matmul(bk[:, 0:4], lhsT=tri, rhs=nl[:, :], start=True, stop=True),
               [r_cst, r_["nl"]], [rbk])
            pe(lambda e, bk=bk: e.matmul(bk[:, 4:8], lhsT=ones128, rhs=nl[:, :], start=True, stop=True),
               [r_cst, r_["nl"]], [rbk])
            dve(lambda e, bk=bk: e.scalar_tensor_tensor(out=gt[:, :], in0=bk[:, 0:4], scalar=-0.5 * math.log(128.0),
                                                 in1=ifs[:, 0:4], op0=ALU.add, op1=ALU.add),
                [rbk, r_["ifs"]], [r_["gt"]])
            act(lambda e: e.activation(out=g4[:, :], in_=gt[:, :], func=AF.Exp), [r_["gt"]], [r_["g4"]])
            act(lambda e, bk=bk: e.activation(out=thrc[:, :], in_=bk[:, 0:4], func=AF.Exp), [rbk], [rthr])
            act(lambda e, bk=bk: e.activation(out=decc[:, :], in_=bk[:, 4:8], func=AF.Exp, scale=-1.0), [rbk], [rdec])
            yield
            for half in range(2):
                bank, rb = rotbank()
                proj_fm(bank, rb, 1024 + half * 512, 4, hTc, rhT)
                act(lambda e, bank=bank, half=half: e.activation(
                    out=qkraw[:, half * 4:half * 4 + 4, 3:131], in_=bank[:, :].rearrange("p (j t) -> p j t", t=128),
                    func=AF.Copy), [rb], [r_["qkraw"]])
                yield
            for h in range(4):
                dve(lambda e, h=h: e.tensor_scalar(out=vtc[:, h, :], in0=vaug[:, h, :], scalar1=g4[:, h:h + 1],
                                                   scalar2=None, op0=ALU.mult), [r_["vaug"], r_["g4"]], [rvt])
                dve(lambda e, h=h: e.tensor_scalar(out=vhc[:, h, :], in0=vaug[:, h, :], scalar1=g4[:, h:h + 1],
                                                   scalar2=decc[:, h:h + 1], op0=ALU.mult, op1=ALU.mult),
                    [r_["vaug"], r_["g4"], rdec], [rvh])
            yield
            for half in range(2):
                bank, rb = rotbank()
                for jj in range(4):
                    j = half * 4 + jj
                    for tap in range(4):
                        pe(lambda e, bank=bank, j=j, jj=jj, tap=tap: e.matmul(
                            bank[:, jj * 128:(jj + 1) * 128], lhsT=dgq[:, j * 4 + tap, :],
                            rhs=qkraw[:, j, tap:tap + 128], start=(tap == 0), stop=False),
                           [r_dgq, r_["qkraw"]], [rb])
                    pe(lambda e, bank=bank, j=j, jj=jj: e.matmul(
                        bank[:, jj * 128:(jj + 1) * 128], lhsT=browb[0:1, j * 128:(j + 1) * 128], rhs=onesb[0:1, :],
                        start=False, stop=True), [r_["browb"], r_["onesb"]], [rb])
                sigmoid_chain(bank[:, :], rb, tq[:, :], r_["tq"])
                dve(lambda e, bank=bank, half=half: e.tensor_tensor(
                    out=qkTc[:, half * 4:half * 4 + 4, :], in0=bank[:, :].rearrange("p (j t) -> p j t", t=128),
                    in1=tq[:, :].rearrange("p (j t) -> p j t", t=128), op=ALU.mult), [rb, r_["tq"]], [rqkT])
                yield
            pool(lambda e: e.tensor_copy(out=qkraw[:, :, 0:3], in_=qkraw[:, :, 128:131]), [r_["qkraw"]], [r_["qkraw"]])
            bk, rbk = rotbank()
            proj_tm(bk, rbk, 2560, 512, hTc, rhT)
            sigmoid_chain(bk[:, :], rbk, soc[:, :], rso)
            yield
            for h in range(4):
                pe(lambda e, h=h: e.transpose(out=T0[:, h, :], in_=qkTc[:, 4 + h, :], identity=identb[:, :]),
                   [rqkT, r_identb], [rT0])
            act(lambda e: e.activation(out=ktmc[:, :].rearrange("p (h d) -> p h d", d=128), in_=T0[:, 0:4, :],
                                       func=AF.Copy), [rT0], [rktm])
            yield
            bk, rbk = rotbank()
            for h in range(4):
                pe(lambda e, h=h, bk=bk: e.matmul(bk[:, h * 128:(h + 1) * 128], lhsT=qkTc[:, 4 + h, :], rhs=qkTc[:, h, :],
                                           start=True, stop=True), [rqkT], [rbk])
            dve(lambda e, bk=bk: e.tensor_tensor(out=PTmc[:, :, :], in0=bk[:, :].rearrange("p (h t) -> p h t", t=128),
                                          in1=mask4.rearrange("p (h t) -> p h t", t=128), op=ALU.mult),
                [rbk, r_cst], [rPTm])
            yield
            bk, rbk = rotbank()
            proj_tm(bk, rbk, 3072, 512, hTc, rhT)
            sigmoid_chain(bk[:, :], rbk, tq[:, :], r_["tq"])
            dve(lambda e, bk=bk: e.tensor_tensor(out=mzc[:, :], in0=bk[:, :], in1=tq[:, :], op=ALU.mult),
                [rbk, r_["tq"]], [rmz])
            dve(lambda e: e.tensor_tensor(out=mzc[:, :], in0=mzc[:, :], in1=prm[:, P_MLG:P_MLG + 512], op=ALU.mult),
                [rmz, r_prm], [rmz])
            yield

        def mlsB(t):
            c = t % 2
            vtc, vhc, soc, mzc, thrc, decc = vt[c], vh[c], so[c], mz[c], thr[c], dec[c]
            qkTc, ktmc, PTmc = qkT[c], ktm[c], PTm[c]
            rvt, rvh, rso, rmz, rthr, rdec = (r_["vt%d" % c], r_["vh%d" % c], r_["so%d" % c], r_["mz%d" % c],
                                               r_["thr%d" % c], r_["dec%d" % c])
            rqkT, rktm, rPTm = r_["qkT%d" % c], r_["ktm%d" % c], r_["PTm%d" % c]
            for g in range(2):
                ob, ro = rotbank()
                for hh in range(2):
                    h = g * 2 + hh
                    c0 = hh * 129
                    pe(lambda e, c0=c0, h=h, ob=ob: e.matmul(ob[:, c0:c0 + 129], lhsT=PTmc[:, h, :], rhs=vtc[:, h, :],
                                                      start=True, stop=False), [rPTm, rvt], [ro])
                    pe(lambda e, c0=c0, h=h, ob=ob: e.matmul(ob[:, c0:c0 + 129], lhsT=qkTc[:, h, :], rhs=Cb[:, h, :],
                                                      start=False, stop=True), [rqkT, r_["Cb"]], [ro])
                act(lambda e, g=g, ob=ob: e.activation(
                    out=den[:, g * 2:g * 2 + 2], in_=ob[:, 0:258].rearrange("p (h d) -> p h d", d=129)[:, :, 128],
                    func=AF.Abs), [ro], [r_["den"]])
                dve(lambda e, g=g: e.tensor_tensor(out=den[:, g * 2:g * 2 + 2], in0=den[:, g * 2:g * 2 + 2],
                                                   in1=thrc[:, g * 2:g * 2 + 2], op=ALU.max),
                    [r_["den"], rthr], [r_["den"]])
                dve(lambda e, g=g: e.reciprocal(out=rden[:, g * 2:g * 2 + 2], in_=den[:, g * 2:g * 2 + 2]),
                    [r_["den"]], [r_["rden"]])
                for hh in range(2):
                    h = g * 2 + hh
                    c0 = hh * 129
                    dve(lambda e, c0=c0, h=h, ob=ob: e.scalar_tensor_tensor(
                        out=hm[:, h * 128:(h + 1) * 128], in0=ob[:, c0:c0 + 128], scalar=rden[:, h:h + 1],
                        in1=soc[:, h * 128:(h + 1) * 128], op0=ALU.mult, op1=ALU.mult),
                        [ro, r_["rden"], rso], [r_["hm"]])
                yield
            for g in range(2):
                ob, ro = rotbank()
                for hh in range(2):
                    h = g * 2 + hh
                    c0 = hh * 129
                    pe(lambda e, c0=c0, h=h, ob=ob: e.matmul(ob[:, c0:c0 + 129], lhsT=ktmc[:, h * 128:(h + 1) * 128],
                                                      rhs=vhc[:, h, :], start=True, stop=True), [rktm, rvh], [ro])
                for hh in range(2):
                    h = g * 2 + hh
                    c0 = hh * 129
                    dve(lambda e, c0=c0, h=h, ob=ob: e.scalar_tensor_tensor(
                        out=Cst[:, h, :], in0=Cst[:, h, :], scalar=decc[:, h:h + 1], in1=ob[:, c0:c0 + 129],
                        op0=ALU.mult, op1=ALU.add), [ro, rdec, r_["Cst"]], [r_["Cst"]])
                yield
            act(lambda e: e.activation(out=Cb[:, :, :], in_=Cst[:, :, :], func=AF.Copy), [r_["Cst"]], [r_["Cb"]])
            dve(lambda e: e.tensor_tensor(out=sq2[:, :], in0=hm[:, :], in1=hm[:, :], op=ALU.mult), [r_["hm"]], [r_["sq2"]])
            dve(lambda e: e.tensor_reduce(out=st4[:, :], in_=sq2[:, :].rearrange("p (g d) -> p g d", d=128),
                                          axis=AX.X, op=ALU.add), [r_["sq2"]], [r_["st4"]])
            rstd_chain(st4[:, :], r_["st4"], 1.0 / 128)
            yield
            for h in range(4):
                dve(lambda e, h=h: e.scalar_tensor_tensor(
                    out=mixed[c][:, 256 + h * 128:256 + (h + 1) * 128], in0=hm[:, h * 128:(h + 1) * 128],
                    scalar=st4[:, h:h + 1], in1=mzc[:, h * 128:(h + 1) * 128], op0=ALU.mult, op1=ALU.mult),
                    [r_["hm"], r_["st4"], rmz], [r_["mixed%d" % c]])
            yield

        def cnvA(t):
            c = t % 2
            hTc, rhT = hT[c], r_["hT%d" % c]
            uTc, ruT = uT[c], r_["uT%d" % c]
            czc, rcz = czs[c], r_["czs%d" % c]
            pool(lambda e: e.tensor_copy(out=uTc[:, :, 0:30], in_=uT[1 - c][:, :, 128:158]),
                 [r_["uT%d" % (1 - c)]], [ruT])
            bank, rb = rotbank()
            proj_fm(bank, rb, 3592, 4, hTc, rhT)
            sigmoid_chain(bank[:, 256:512], rb, tcz[:, :], r_["tcz"])
            dve(lambda e, bank=bank: e.tensor_tensor(out=uTc[:, :, 30:158],
                                                     in0=bank[:, 0:256].rearrange("p (a b) -> p a b", b=128),
                                                     in1=tcz[:, :].rearrange("p (a b) -> p a b", b=128), op=ALU.mult),
                [rb, r_["tcz"]], [ruT])
            yield
            bank, rb = rotbank()
            proj_tm(bank, rb, 4104, 256, hTc, rhT)
            sigmoid_chain(bank[:, 0:256], rb, tcz2[:, :], r_["tcz"])
            dve(lambda e, bank=bank: e.tensor_tensor(out=czc[:, :], in0=bank[:, 0:256], in1=tcz2[:, :], op=ALU.mult),
                [rb, r_["tcz"]], [rcz])
            yield

        def cnvB(t):
            c = t % 2
            uTc, ruT = uT[c], r_["uT%d" % c]
            czc, rcz = czs[c], r_["czs%d" % c]
            for ct in range(2):
                for j in range(31):
                    pe(lambda e, ct=ct, j=j: e.matmul(
                        FB[0][:, ct * 128:(ct + 1) * 128], lhsT=uTc[:, ct, j:j + 128], rhs=dgc[:, ct * 31 + j, :],
                        start=(j == 0), stop=(j == 30)), [ruT, r_dgc], [rF[0]])
                    if (ct * 31 + j) % 6 == 5:
                        yield
            dve(lambda e: e.tensor_tensor(out=uc[:, :], in0=FB[0][:, 0:256], in1=prm[:, P_DWB:P_DWB + 256],
                                          op=ALU.add), [rF[0], r_prm], [r_["uc"]])
            dve(lambda e: e.bn_stats(out=bst[:, :], in_=uc[:, :]), [r_["uc"]], [r_["bst"]])
            dve(lambda e: e.bn_aggr(out=mv[:, :], in_=bst[:, :]), [r_["bst"]], [r_["mv"]])
            act(lambda e: e.activation(out=rstd1[:, :], in_=mv[:, 1:2], func=AF.Ln, bias=EPS), [r_["mv"]], [r_["rstd1"]])
            act(lambda e: e.activation(out=rstd1[:, :], in_=rstd1[:, :], func=AF.Exp, scale=-0.5),
                [r_["rstd1"]], [r_["rstd1"]])
            yield
            dve(lambda e: e.tensor_scalar(out=yln[:, :], in0=uc[:, :], scalar1=mv[:, 0:1], scalar2=rstd1[:, 0:1],
                                          op0=ALU.subtract, op1=ALU.mult), [r_["uc"], r_["mv"], r_["rstd1"]], [r_["yln"]])
            dve(lambda e: e.tensor_tensor(out=yln[:, :], in0=yln[:, :], in1=prm[:, P_LNG:P_LNG + 256], op=ALU.mult),
                [r_["yln"], r_prm], [r_["yln"]])
            dve(lambda e: e.tensor_tensor(out=yln[:, :], in0=yln[:, :], in1=prm[:, P_LNB:P_LNB + 256], op=ALU.add),
                [r_["yln"], r_prm], [r_["yln"]])
            sigmoid_chain(yln[:, :], r_["yln"], tyl2[:, :], r_["tyl2"])
            yield
            dve(lambda e: e.tensor_tensor(out=tyl2[:, :], in0=tyl2[:, :], in1=yln[:, :], op=ALU.mult),
                [r_["tyl2"], r_["yln"]], [r_["tyl2"]])
            dve(lambda e: e.tensor_tensor(out=mixed[c][:, 768:1024], in0=tyl2[:, :], in1=czc[:, :], op=ALU.mult),
                [r_["tyl2"], rcz], [r_["mixed%d" % c]])
            yield

        def tail(t, l=l, dst_d=dst_d):
            b = t % 4
            c = t % 2
            X, rX = xt[b], r_x[b]
            for k in range(8):
                pe(lambda e, k=k: e.transpose(out=T0[:, k, :], in_=mixed[c][:, k * 128:(k + 1) * 128], identity=identb[:, :]),
                   [r_["mixed%d" % c], r_identb], [rT0])
            act(lambda e: e.activation(out=mT[:, 0:4, :], in_=T0[:, 0:4, :], func=AF.Copy), [rT0], [r_["mT"]])
            dve(lambda e: e.tensor_copy(out=mT[:, 4:8, :], in_=T0[:, 4:8, :]), [rT0], [r_["mT"]])
            yield
            for nb in range(2):
                bank, rb = rotbank()
                for k in range(8):
                    pe(lambda e, bank=bank, k=k, nb=nb: e.matmul(bank[:, :], lhsT=mT[:, k, :],
                                                                 rhs=w_out[:, k, nb * 512:(nb + 1) * 512],
                                                                 start=(k == 0), stop=(k == 7)),
                       [r_["mT"], r_wout], [rb])
                dve(lambda e, bank=bank, nb=nb: e.tensor_tensor(
                    out=X[:, nb * 512:(nb + 1) * 512], in0=bank[:, :], in1=X[:, nb * 512:(nb + 1) * 512], op=ALU.add),
                    [rb], [rX])
                yield
            wr = [r_mid[t]] if l < L - 1 else []
            sch.dma("sp", "d_o%d" % b,
                    lambda e: e.dma_start(out=dst_d[t * 128:(t + 1) * 128, :], in_=X[:, :]), [rX], wr)
            yield

        def drive(gens):
            gens = list(gens)
            while gens:
                for g in list(gens):
                    try:
                        next(g)
                    except StopIteration:
                        gens.remove(g)

        load(0)
        if NT > 1:
            load(1)
        drive([front(0)])
        for st in range(NT + 2):
            gens = []
            if 1 <= st <= NT:
                gens.append(cnvB(st - 1))
            if 2 <= st <= NT + 1:
                gens.append(tail(st - 2))
            if st < NT:
                gens += [cnvA(st), attA(st), mlsA(st)]
            if st + 1 < NT:
                gens.append(front(st + 1))
            if 1 <= st <= NT:
                gens += [mlsB(st - 1), attB(st - 1)]
            drive(gens)
            if st + 2 < NT:
                load(st + 2)
    final_waits = [(k, v) for k, v in sch.dma_cnt.items() if k.startswith("d_o")]

    with nc.Block() as block:
        ctxs = sch.emit(nc, block, final_waits)
    return nc


def _host_prep(inputs, L):
    f = np.float32
    prm = np.zeros((L, 128, NPRM), f)
    attb = np.zeros((L, 128, 20, 128), f)
    ql = np.arange(128)[None, :]
    kl = np.arange(128)[:, None]
    for l in range(L):
        prm[l, :, P_GIN:P_GIN + 8] = inputs["norm_g"][l].reshape(8, 128).T
        prm[l, :, P_QG:P_QG + 64] = inputs["att_q_g"][l][None, :]
        prm[l, :, P_KG:P_KG + 64] = inputs["att_k_g"][l][None, :]
        prm[l, :, P_MLG:P_MLG + 512] = inputs["ml_out_g"][l][None, :]
        prm[l, :, P_LNG:P_LNG + 256] = inputs["cv_ln_g"][l][None, :]
        prm[l, :, P_LNB:P_LNB + 256] = inputs["cv_ln_b"][l][None, :]
        prm[l, :, P_DWB:P_DWB + 256] = inputs["cv_dw_b"][l][None, :]
        prm[l, :, P_BIF:P_BIF + 4] = inputs["ml_b_i"][l][None, :]
        prm[l, :, P_BIF + 4:P_BIF + 8] = inputs["ml_b_f"][l][None, :]
        w = inputs["ml_qk_conv_w"][l]
        prm[l, :, P_QKCW:P_QKCW + 32] = w.reshape(4, 8, 128).transpose(2, 1, 0).reshape(128, 32)
        prm[l, :, P_QKCB:P_QKCB + 8] = inputs["ml_qk_conv_b"][l].reshape(8, 128).T
        w = inputs["cv_dw_w"][l]
        prm[l, :, P_DWW:P_DWW + 62] = w.reshape(31, 2, 128).transpose(2, 1, 0).reshape(128, 62)
        rb = inputs["att_rel_bias"][l]
        for j in range(5):
            rel = ql - kl + (8 - 2 * j) * 64
            idx = np.clip(rel, -128, 128) + 128
            dd = ql // 64 - kl // 64 + 8 - 2 * j
            valid = (dd >= 0) & (dd <= 8)
            for h in range(4):
                attb[l, :, h * 5 + j, :] = np.where(valid, rb[h][idx], f(NEGB))
    cst = np.zeros((128, 896), f)
    cst[:, 0:128] = np.eye(128, dtype=f)
    tri = (np.arange(128)[:, None] <= np.arange(128)[None, :]).astype(f)
    cst[:, 128:256] = tri
    cst[:, 256:768] = np.tile(tri, (1, 4))
    cst[:, 768:896] = 1.0
    rowp = np.ascontiguousarray(inputs["ml_qk_conv_b"][:L].reshape(L, 1, 1024), dtype=f)
    return prm, attb.reshape(L, 128, 2560), cst, rowp


def kernel(**inputs):
    inputs = {k: np.asarray(v) for k, v in inputs.items()}
    x = inputs["x"]
    B, S, _ = x.shape
    L = inputs["w_in"].shape[0]
    prm, attb, cst, rowp = _host_prep(inputs, L)
    nc = build_nc(S, L)
    w_in = np.ascontiguousarray(inputs["w_in"], dtype=np.float32)
    w_out = np.ascontiguousarray(inputs["w_out"], dtype=np.float32)
    in_maps = [{"x": np.ascontiguousarray(x[i]), "w_in": w_in, "w_out": w_out, "prm": prm, "attb": attb, "cst": cst,
                "rowp": rowp} for i in range(B)]
    res = run_bass_kernel_spmd(nc, in_maps, core_ids=list(range(B)))
    return np.stack([np.asarray(r["out"]) for r in res.results], axis=0).astype(np.float32)
```

```python
import math
import numpy as np
import concourse.bass as bass
import concourse.mybir as mybir
from concourse.bass_utils import run_bass_kernel_spmd

F32 = mybir.dt.float32
BF16 = mybir.dt.bfloat16
AF = mybir.ActivationFunctionType
ALU = mybir.AluOpType
AX = mybir.AxisListType

D = 1024
DIN = 4360
EPS = 1e-6
NEGB = -30000.0
P_GIN, P_QG, P_KG, P_MLG, P_LNG, P_LNB, P_DWB, P_BIF, P_QKCW, P_QKCB, P_DWW = (
    0, 8, 72, 136, 648, 904, 1160, 1416, 1424, 1456, 1464)
NPRM = 1526


class Res:
    __slots__ = ("name", "w", "r", "excl")

    def __init__(self, name, excl=False):
        self.name = name
        self.w = None
        self.r = {}
        self.excl = excl


class Sched:
    ENGS = ("pe", "act", "dve", "pool", "sp")

    def __init__(self):
        self.streams = {e: [] for e in self.ENGS}
        self.cnt = {e: 0 for e in self.ENGS}
        self.known = {e: {} for e in self.ENGS}
        self.dma_cnt = {}

    def _deps(self, eng, reads, writes):
        deps = {}

        def add(k, v):
            if deps.get(k, 0) < v:
                deps[k] = v
        for r in reads:
            if r.w is not None:
                add(*r.w)
            if r.excl:
                for k, v in r.r.items():
                    if k != eng:
                        add(k, v)
        for w in writes:
            if w.w is not None:
                add(*w.w)
            for k, v in w.r.items():
                if k != eng:
                    add(k, v)
        waits = []
        for k, v in deps.items():
            if k == eng and eng == "pe":
                continue
            if self.known[eng].get(k, 0) >= v:
                continue
            self.known[eng][k] = v
            waits.append((k, v))
        return waits

    @staticmethod
    def _mark(tok, reads, writes):
        k, v = tok
        for r in reads:
            if r.r.get(k, 0) < v:
                r.r[k] = v
        for w in writes:
            w.w = tok
            w.r = {}

    def op(self, eng, fn, reads=(), writes=()):
        waits = self._deps(eng, reads, writes)
        self.cnt[eng] += 1
        tok = (eng, self.cnt[eng])
        self._mark(tok, reads, writes)
        self.streams[eng].append([waits, fn, tok, None])

    def dma(self, q, sem, fn, reads=(), writes=()):
        waits = self._deps(q, reads, writes)
        self.dma_cnt[sem] = self.dma_cnt.get(sem, 0) + 16
        tok = (sem, self.dma_cnt[sem])
        self._mark(tok, reads, writes)
        self.streams[q].append([waits, fn, None, (sem, 16)])

    def emit(self, nc, block, final_waits):
        waited = {e: set() for e in self.ENGS}
        for e in self.ENGS:
            for waits, fn, tok, dm in self.streams[e]:
                for k, v in waits:
                    if k in waited:
                        waited[k].add(v)
        rank = {}
        for e in self.ENGS:
            srt = sorted(waited[e])
            rank[e] = {v: i + 1 for i, v in enumerate(srt)}
        sem_names = list(self.ENGS) + sorted(self.dma_cnt.keys())
        sems = {}
        ctxs = []
        for n in sem_names:
            c = nc.semaphore("s_" + n)
            sems[n] = c.__enter__()
            ctxs.append(c)

        def run(e, eng):
            for waits, fn, tok, dm in self.streams[e]:
                for k, v in waits:
                    if k in rank:
                        eng.wait_ge(sems[k], rank[k][v])
                    else:
                        eng.wait_ge(sems[k], v)
                ins = fn(eng)
                if dm is not None:
                    ins.then_inc(sems[dm[0]], dm[1])
                elif tok[1] in rank[e]:
                    ins.then_inc(sems[e], 1)
            if e == "sp":
                for k, v in final_waits:
                    eng.wait_ge(sems[k], v)

        @block.tensor
        def _(eng):
            run("pe", eng)

        @block.scalar
        def _(eng):
            run("act", eng)

        @block.vector
        def _(eng):
            run("dve", eng)

        @block.gpsimd
        def _(eng):
            run("pool", eng)

        @block.sync
        def _(eng):
            run("sp", eng)
        return ctxs


def build_nc(S, L):
    NT = S // 128
    nc = bass.Bass("TRN2", target_bir_lowering=False)
    x_d = nc.dram_tensor("x", [S, D], F32, kind="ExternalInput").ap()
    win_d = nc.dram_tensor("w_in", [L, D, DIN], F32, kind="ExternalInput").ap()
    wout_d = nc.dram_tensor("w_out", [L, D, D], F32, kind="ExternalInput").ap()
    prm_d = nc.dram_tensor("prm", [L, 128, NPRM], F32, kind="ExternalInput").ap()
    attb_d = nc.dram_tensor("attb", [L, 128, 2560], F32, kind="ExternalInput").ap()
    cst_d = nc.dram_tensor("cst", [128, 896], F32, kind="ExternalInput").ap()
    rowp_d = nc.dram_tensor("rowp", [L, 1, 1024], F32, kind="ExternalInput").ap()
    out_d = nc.dram_tensor("out", [S, D], F32, kind="ExternalOutput").ap()
    mid_d = nc.dram_tensor("xmid", [S, D], F32, kind="Internal").ap() if L > 1 else None

    sch = Sched()
    stack = []

    def sb(name, shape, dt):
        c = nc.sbuf_tensor(name, shape, dt)
        t = c.__enter__()
        stack.append(c)
        return t

    def ps(name, shape, dt):
        c = nc.psum_tensor(name, shape, dt)
        t = c.__enter__()
        stack.append(c)
        return t

    w_in = sb("w_in_sb", [128, 8, DIN], BF16)
    w_out = sb("w_out_sb", [128, 8, D], BF16)
    prm = sb("prm_sb", [128, NPRM], F32)
    cst = sb("cst_sb", [128, 896], F32)
    identb = sb("identb", [128, 128], BF16)
    attb = sb("attb_sb", [128, 20, 128], BF16)
    dgc = sb("dgc", [128, 62, 128], BF16)
    dgq = sb("dgq", [128, 32, 128], BF16)
    negqb = sb("negqb", [128, 8], F32)
    xt = [sb("xt%d" % i, [128, D], F32) for i in range(4)]
    stg = [xt[2], xt[3]]
    browb = sb("browb", [1, 1024], BF16)
    onesb = sb("onesb", [1, 128], BF16)
    st1 = sb("st1", [128, 8], F32)
    hb = [sb("hb0", [128, D], BF16)] * 2
    hT = [sb("hT%d" % i, [128, 8, 128], BF16) for i in range(2)]
    qk32 = sb("qk32", [128, 512], F32)
    sq = sb("sq", [128, 512], BF16)
    sq2 = sb("sq2", [128, 512], BF16)
    st8 = sb("st8", [128, 8], F32)
    qkn = sb("qkn", [128, 512], BF16)
    qT = [sb("qT%d" % i, [128, 2, 128], BF16) for i in range(2)]
    kTr = [sb("kTr%d" % i, [128, 2, 128], BF16) for i in range(6)]
    vr = [sb("vr%d" % i, [128, 4, 65], BF16) for i in range(6)]
    zs = [sb("zs%d" % i, [128, 256], BF16) for i in range(2)]
    tz = sb("tz", [128, 256], F32)
    PT = [sb("PT%d" % i, [128, 5, 128], BF16) for i in range(2)]
    rs4 = sb("rs4", [128, 4], F32)
    mixed = [sb("mixed%d" % i, [128, D], BF16) for i in range(2)]
    mT = sb("mT", [128, 8, 128], BF16)
    vaug = sb("vaug", [128, 4, 129], F32)
    vt = [sb("vt%d" % i, [128, 4, 129], BF16) for i in range(2)]
    vh = [sb("vh%d" % i, [128, 4, 129], BF16) for i in range(2)]
    so = [sb("so%d" % i, [128, 512], F32) for i in range(2)]
    mz = [sb("mz%d" % i, [128, 512], F32) for i in range(2)]
    ifs = sb("ifs", [128, 8], F32)
    nl = sb("nl", [128, 4], F32)
    gt = sb("gt", [128, 4], F32)
    g4 = sb("g4", [128, 4], F32)
    thr = [sb("thr%d" % i, [128, 4], F32) for i in range(2)]
    dec = [sb("dec%d" % i, [128, 4], F32) for i in range(2)]
    den = sb("den", [128, 4], F32)
    rden = sb("rden", [128, 4], F32)
    qkraw = sb("qkraw", [128, 8, 131], BF16)
    tq = sb("tq", [128, 512], F32)
    qkT = [sb("qkT%d" % i, [128, 8, 128], BF16) for i in range(2)]
    ktm = [sb("ktm%d" % i, [128, 512], BF16) for i in range(2)]
    PTm = [sb("PTm%d" % i, [128, 4, 128], BF16) for i in range(2)]
    Cst = sb("Cst", [128, 4, 129], F32)
    Cb = sb("Cb", [128, 4, 129], BF16)
    hm = sb("hm", [128, 512], F32)
    st4 = sb("st4", [128, 4], F32)
    uT = [sb("uT%d" % i, [128, 2, 158], BF16) for i in range(2)]
    uc = sb("uc", [128, 256], F32)
    bst = sb("bst", [128, 6], F32)
    mv = sb("mv", [128, 2], F32)
    rstd1 = sb("rstd1", [128, 1], F32)
    yln = sb("yln", [128, 256], F32)
    czs = [sb("czs%d" % i, [128, 256], BF16) for i in range(2)]
    tcz = sb("tcz", [128, 256], F32)
    tcz2 = tcz
    tyl2 = sb("tyl2", [128, 256], F32)
    T0 = ps("T0", [128, 8, 128], BF16)
    FB = [ps("F%d" % i, [128, 512], F32) for i in range(7)]

    R = {}

    def res(name, excl=False):
        if name not in R:
            R[name] = Res(name, excl)
        return R[name]
    rT0 = res("T0", True)
    rF = [res("F%d" % i, True) for i in range(7)]
    rot = [0]

    def rotbank():
        i = 1 + rot[0] % 5
        rot[0] += 1
        return FB[i], rF[i]

    def act(fn, reads, writes):
        sch.op("act", fn, reads, writes)

    def dve(fn, reads, writes):
        sch.op("dve", fn, reads, writes)

    def pool(fn, reads, writes):
        sch.op("pool", fn, reads, writes)

    def pe(fn, reads, writes):
        sch.op("pe", fn, reads, writes)

    def sigmoid_chain(src_ap, src_res, tmp_ap, tmp_res, bias_neg=None):
        if bias_neg is None:
            act(lambda e: e.activation(out=tmp_ap, in_=src_ap, func=AF.Exp, scale=-1.0), [src_res], [tmp_res])
        else:
            act(lambda e: e.activation(out=tmp_ap, in_=src_ap, func=AF.Exp, scale=-1.0, bias=bias_neg),
                [src_res], [tmp_res])
        act(lambda e: e.activation(out=tmp_ap, in_=tmp_ap, func=AF.Ln, bias=1.0), [tmp_res], [tmp_res])
        act(lambda e: e.activation(out=tmp_ap, in_=tmp_ap, func=AF.Exp, scale=-1.0), [tmp_res], [tmp_res])

    def rstd_chain(ap, r, scale):
        act(lambda e: e.activation(out=ap, in_=ap, func=AF.Ln, scale=scale, bias=EPS), [r], [r])
        act(lambda e: e.activation(out=ap, in_=ap, func=AF.Exp, scale=-0.5), [r], [r])

    r_cst, r_identb = res("cst"), res("identb")
    sch.dma("sp", "d_cst", lambda e: e.dma_start(out=cst[:, :], in_=cst_d[:, :]), [], [r_cst])
    dve(lambda e: e.tensor_copy(out=identb[:, :], in_=cst[:, 0:128]), [r_cst], [r_identb])
    ident32 = cst[:, 0:128]
    tri = cst[:, 128:256]
    mask4 = cst[:, 256:768]
    ones128 = cst[:, 768:896]

    r_win, r_wout, r_prm, r_attb = res("w_in"), res("w_out"), res("prm"), res("attb")
    r_stg = [res("xt2"), res("xt3")]
    r_dgc, r_dgq, r_negqb = res("dgc"), res("dgq"), res("negqb")
    r_x = [res("xt0"), res("xt1"), res("xt2"), res("xt3")]
    r_mid = [res("mid%d" % t) for t in range(NT)]
    r_kT = [res("kT%d" % i) for i in range(6)]
    r_v = [res("v%d" % i) for i in range(6)]
    names = ["qT0", "qT1", "zs0", "zs1", "uT0", "uT1", "czs0", "czs1", "browb", "onesb", "mixed0", "mixed1", "vt0", "vt1", "vh0", "vh1", "so0", "so1", "mz0", "mz1", "thr0", "thr1",
             "dec0", "dec1", "qkT0", "qkT1", "ktm0", "ktm1", "PTm0", "PTm1", "st1", "hb0", "hT0", "hT1", "qk32", "sq", "sq2", "st8", "qkn", "qT", "zs", "tz", "PT0", "PT1", "rs4", "mixed", "mT",
             "vaug", "vt", "vh", "so", "mz", "ifs", "nl", "gt", "g4", "thr", "dec", "den", "rden",
             "qkraw", "tq", "qkT", "ktm", "PTm", "Cst", "Cb", "hm", "st4", "uT", "tcb", "uc", "bst",
             "mv", "rstd1", "yln", "tyl2", "tcz"]
    r_ = {n: res(n) for n in names}
    dve(lambda e: e.tensor_copy(out=onesb[:, :], in_=cst[0:1, 768:896]), [r_cst], [r_["onesb"]])
    stg_i = [0]

    pool(lambda e: e.memset(vaug[:, :, :], 1.0), [], [r_["vaug"]])
    for i in range(6):
        pool(lambda e, i=i: e.memset(vr[i][:, :, :], 1.0), [], [r_v[i]])

    final_waits = []

    for l in range(L):
        src_d = x_d if l == 0 else mid_d
        dst_d = out_d if l == L - 1 else mid_d
        sch.dma("sp", "d_prm", lambda e, l=l: e.dma_start(out=prm[:, :], in_=prm_d[l, :, :]), [], [r_prm])
        def stage(src_ap, ncols, cast_fn):
            s = stg_i[0] % 2
            stg_i[0] += 1
            sch.dma("sp", "d_stg%d" % s, lambda e, s=s: e.dma_start(out=stg[s][:, 0:ncols], in_=src_ap),
                    [], [r_stg[s]])
            cast_fn(stg[s][:, 0:ncols], r_stg[s], stg_i[0] % 2 == 0)

        s0 = stg_i[0] % 2
        stg_i[0] += 1
        sch.dma("sp", "d_stg%d" % s0, lambda e, l=l, s0=s0: e.dma_start(out=stg[s0][0:1, :], in_=rowp_d[l, :, :]),
                [], [r_stg[s0]])
        dve(lambda e, s0=s0: e.tensor_copy(out=browb[:, :], in_=stg[s0][0:1, :]), [r_stg[s0]], [r_["browb"]])
        for c0, n in ((0, 1024), (1024, 1024), (2048, 512)):
            def cast_b(src, rs, use_act, c0=c0, n=n):
                dve(lambda e: e.tensor_copy(out=attb[:, c0 // 128:(c0 + n) // 128, :],
                                            in_=src.rearrange("p (a b) -> p a b", b=128)), [rs], [r_attb])
            stage(attb_d[l, :, c0:c0 + n], n, cast_b)
        for k in range(8):
            for c0, n in ((0, 1024), (1024, 1024), (2048, 1024), (3072, 1024), (4096, 264)):
                def cast_w(src, rs, use_act, k=k, c0=c0, n=n):
                    if use_act:
                        act(lambda e: e.activation(out=w_in[:, k, c0:c0 + n], in_=src, func=AF.Copy,
                                                   scale=prm[:, P_GIN + k:P_GIN + k + 1]), [rs, r_prm], [r_win])
                    else:
                        dve(lambda e: e.tensor_scalar(out=w_in[:, k, c0:c0 + n], in0=src,
                                                      scalar1=prm[:, P_GIN + k:P_GIN + k + 1], scalar2=None,
                                                      op0=ALU.mult), [rs, r_prm], [r_win])
                stage(win_d[l, k * 128:(k + 1) * 128, c0:c0 + n], n, cast_w)
        for k in range(8):
            def cast_o(src, rs, use_act, k=k):
                if use_act:
                    act(lambda e: e.activation(out=w_out[:, k, :], in_=src, func=AF.Copy), [rs], [r_wout])
                else:
                    dve(lambda e: e.tensor_copy(out=w_out[:, k, :], in_=src), [rs], [r_wout])
            stage(wout_d[l, k * 128:(k + 1) * 128, :], 1024, cast_o)
        for i in range(62):
            pool(lambda e, i=i: e.tensor_scalar(out=dgc[:, i, :], in0=ident32, scalar1=prm[:, P_DWW + i:P_DWW + i + 1],
                                                scalar2=None, op0=ALU.mult),
                 [r_cst, r_prm], [r_dgc])
        for i in range(32):
            pool(lambda e, i=i: e.tensor_scalar(out=dgq[:, i, :], in0=ident32,
                                                scalar1=prm[:, P_QKCW + i:P_QKCW + i + 1],
                                                scalar2=None, op0=ALU.mult),
                 [r_cst, r_prm], [r_dgq])
        dve(lambda e: e.tensor_scalar(out=negqb[:, :], in0=prm[:, P_QKCB:P_QKCB + 8], scalar1=-1.0, scalar2=None,
                                      op0=ALU.mult), [r_prm], [r_negqb])
        pool(lambda e: e.memset(Cst[:, :, :], 0.0), [], [r_["Cst"]])
        pool(lambda e: e.memset(Cb[:, :, :], 0.0), [], [r_["Cb"]])
        pool(lambda e: e.memset(uT[1][:, :, 128:158], 0.0), [], [r_["uT1"]])
        pool(lambda e: e.memset(qkraw[:, :, 0:3], 0.0), [], [r_["qkraw"]])

        def load(t, l=l, src_d=src_d):
            b = t % 4
            rd = [r_mid[t]] if l > 0 else []
            sch.dma("sp", "d_x%d" % b,
                    lambda e: e.dma_start(out=xt[b][:, :], in_=src_d[t * 128:(t + 1) * 128, :]), rd, [r_x[b]])

        def proj_tm(bank, rb, c0, n, hTc, rhT):
            for k in range(8):
                pe(lambda e, k=k: e.matmul(bank[:, 0:n], lhsT=hTc[:, k, :], rhs=w_in[:, k, c0:c0 + n],
                                           start=(k == 0), stop=(k == 7)), [rhT, r_win], [rb])

        def proj_fm(bank, rb, c0, ntile, hTc, rhT):
            for j in range(ntile):
                for k in range(8):
                    pe(lambda e, k=k, j=j: e.matmul(bank[:, j * 128:(j + 1) * 128],
                                                    lhsT=w_in[:, k, c0 + j * 128:c0 + (j + 1) * 128],
                                                    rhs=hTc[:, k, :], start=(k == 0), stop=(k == 7)),
                       [rhT, r_win], [rb])

        def front(t):
            c = t % 2
            X, rX = xt[t % 4], r_x[t % 4]
            hbc, hTc = hb[c], hT[c]
            rhb, rhT = r_["hb0"], r_["hT%d" % c]
            dve(lambda e: e.scalar_tensor_tensor(out=hbc[:, :], in0=X[:, :], scalar=1.0, in1=X[:, :],
                                                 op0=ALU.mult, op1=ALU.mult, accum_out=st1[:, 0:1]),
                [rX], [rhb, r_["st1"]])
            rstd_chain(st1[:, 0:1], r_["st1"], 1.0 / D)
            dve(lambda e: e.tensor_scalar(out=hbc[:, :], in0=X[:, :], scalar1=st1[:, 0:1], scalar2=None,
                                          op0=ALU.mult), [rX, r_["st1"]], [rhb])
            yield
            for k in range(8):
                pe(lambda e, k=k: e.transpose(out=T0[:, k, :], in_=hbc[:, k * 128:(k + 1) * 128], identity=identb[:, :]),
                   [rhb, r_identb], [rT0])
            act(lambda e: e.activation(out=hTc[:, 0:4, :], in_=T0[:, 0:4, :], func=AF.Copy), [rT0], [rhT])
            dve(lambda e: e.tensor_copy(out=hTc[:, 4:8, :], in_=T0[:, 4:8, :]), [rT0], [rhT])
            yield

        def attA(t):
            c = t % 2
            hTc, rhT = hT[c], r_["hT%d" % c]
            slot = t % 6
            zsc, rzs = zs[c], r_["zs%d" % c]
            qTc, rqT = qT[c], r_["qT%d" % c]
            bank, rb = rotbank()
            proj_tm(bank, rb, 0, 512, hTc, rhT)
            act(lambda e, bank=bank: e.activation(out=qk32[:, :], in_=bank[:, :], func=AF.Copy), [rb], [r_["qk32"]])
            yield
            dve(lambda e: e.tensor_tensor(out=sq[:, :], in0=qk32[:, :], in1=qk32[:, :], op=ALU.mult),
                [r_["qk32"]], [r_["sq"]])
            dve(lambda e: e.tensor_reduce(out=st8[:, :], in_=sq[:, :].rearrange("p (g d) -> p g d", d=64),
                                          axis=AX.X, op=ALU.add), [r_["sq"]], [r_["st8"]])
            rstd_chain(st8[:, :], r_["st8"], 1.0 / 64)
            dve(lambda e: e.tensor_scalar(out=st8[:, 0:4], in0=st8[:, 0:4], scalar1=0.125, scalar2=None, op0=ALU.mult),
                [r_["st8"]], [r_["st8"]])
            yield
            bank, rb = rotbank()
            proj_tm(bank, rb, 512, 512, hTc, rhT)
            act(lambda e, bank=bank: e.activation(
                out=vr[slot][:, :, 0:64], in_=bank[:, 0:256].rearrange("p (h d) -> p h d", d=64), func=AF.Copy),
                [rb], [r_v[slot]])
            sigmoid_chain(bank[:, 256:512], rb, tz[:, 0:256], r_["tz"])
            dve(lambda e, bank=bank: e.tensor_tensor(out=zsc[:, :], in0=bank[:, 256:512], in1=tz[:, 0:256], op=ALU.mult),
                [rb, r_["tz"]], [rzs])
            yield
            for g in range(8):
                gcol = P_QG if g < 4 else P_KG
                dve(lambda e, g=g, gcol=gcol: e.scalar_tensor_tensor(
                    out=qkn[:, g * 64:(g + 1) * 64], in0=qk32[:, g * 64:(g + 1) * 64], scalar=st8[:, g:g + 1],
                    in1=prm[:, gcol:gcol + 64], op0=ALU.mult, op1=ALU.mult),
                    [r_["qk32"], r_["st8"], r_prm], [r_["qkn"]])
            yield
            for i in range(4):
                pe(lambda e, i=i: e.transpose(out=T0[:, i, :], in_=qkn[:, i * 128:(i + 1) * 128], identity=identb[:, :]),
                   [r_["qkn"], r_identb], [rT0])
            act(lambda e: e.activation(out=qTc[:, :, :], in_=T0[:, 0:2, :], func=AF.Copy), [rT0], [rqT])
            dve(lambda e: e.tensor_copy(out=kTr[slot][:, :, :], in_=T0[:, 2:4, :]), [rT0], [r_kT[slot]])
            yield

        def attB(t):
            c = t % 2
            zsc, rzs = zs[c], r_["zs%d" % c]
            qTc, rqT = qT[c], r_["qT%d" % c]
            jmin = max(0, 4 - t)

            def scores(h):
                p, half = h // 2, h % 2
                lo = 64 * half
                PTc, rPT = PT[h % 2], r_["PT%d" % (h % 2)]
                bx, rbx = rotbank()
                by, rby = rotbank()
                for j in range(jmin, 5):
                    ks = (t - 4 + j) % 6
                    if j < 4:
                        o_ap, ro = bx[:, j * 128:(j + 1) * 128], rbx
                    else:
                        o_ap, ro = by[:, 0:128], rby
                    pe(lambda e, o_ap=o_ap, ks=ks: e.matmul(
                        o_ap, lhsT=kTr[ks][lo:lo + 64, p, :], rhs=qTc[lo:lo + 64, p, :], start=True, stop=False),
                       [r_kT[ks], rqT], [ro])
                    pe(lambda e, o_ap=o_ap, j=j: e.matmul(
                        o_ap, lhsT=identb[:, :], rhs=attb[:, h * 5 + j, :], start=False, stop=True),
                       [r_identb, r_attb], [ro])
                if jmin < 4:
                    act(lambda e: e.activation(
                        out=PTc[:, jmin:4, :], in_=bx[:, jmin * 128:512].rearrange("p (j q) -> p j q", q=128),
                        func=AF.Exp), [rbx], [rPT])
                act(lambda e: e.activation(out=PTc[:, 4, :], in_=by[:, 0:128], func=AF.Exp), [rby], [rPT])

            def pv(h):
                PTc, rPT = PT[h % 2], r_["PT%d" % (h % 2)]
                for j in range(jmin, 5):
                    ks = (t - 4 + j) % 6
                    pe(lambda e, ks=ks, j=j: e.matmul(
                        FB[6][:, h * 65:(h + 1) * 65], lhsT=PTc[:, j, :], rhs=vr[ks][:, h, :],
                        start=(j == jmin), stop=(j == 4)),
                       [rPT, r_v[ks]], [rF[6]])

            scores(0)
            yield
            for h in range(4):
                if h + 1 < 4:
                    scores(h + 1)
                    yield
                pv(h)
                yield
            dve(lambda e: e.reciprocal(out=rs4[:, :],
                                       in_=FB[6][:, 0:260].rearrange("p (h d) -> p h d", d=65)[:, :, 64]),
                [rF[6]], [r_["rs4"]])
            for h in range(4):
                dve(lambda e, h=h: e.scalar_tensor_tensor(
                    out=mixed[c][:, h * 64:(h + 1) * 64], in0=FB[6][:, h * 65:h * 65 + 64],
                    scalar=rs4[:, h:h + 1], in1=zsc[:, h * 64:(h + 1) * 64], op0=ALU.mult, op1=ALU.mult),
                    [rF[6], r_["rs4"], rzs], [r_["mixed%d" % c]])
            yield

        def mlsA(t):
            c = t % 2
            hTc, rhT = hT[c], r_["hT%d" % c]
            vtc, vhc, soc, mzc, thrc, decc = vt[c], vh[c], so[c], mz[c], thr[c], dec[c]
            qkTc, ktmc, PTmc = qkT[c], ktm[c], PTm[c]
            rvt, rvh, rso, rmz, rthr, rdec = (r_["vt%d" % c], r_["vh%d" % c], r_["so%d" % c], r_["mz%d" % c],
                                               r_["thr%d" % c], r_["dec%d" % c])
            rqkT, rktm, rPTm = r_["qkT%d" % c], r_["ktm%d" % c], r_["PTm%d" % c]
            bk, rbk = rotbank()
            proj_tm(bk, rbk, 2048, 512, hTc, rhT)
            act(lambda e, bk=bk: e.activation(out=vaug[:, :, 0:128],
                                       in_=bk[:, :].rearrange("p (h d) -> p h d", d=128), func=AF.Copy),
                [rbk], [r_["vaug"]])
            yield
            bk, rbk = rotbank()
            proj_tm(bk, rbk, 3584, 8, hTc, rhT)
            dve(lambda e, bk=bk: e.tensor_tensor(out=ifs[:, :], in0=bk[:, 0:8], in1=prm[:, P_BIF:P_BIF + 8],
                                          op=ALU.add), [rbk, r_prm], [r_["ifs"]])
            act(lambda e: e.activation(out=nl[:, :], in_=ifs[:, 4:8], func=AF.Exp, scale=-1.0), [r_["ifs"]], [r_["nl"]])
            act(lambda e: e.activation(out=nl[:, :], in_=nl[:, :], func=AF.Ln, bias=1.0), [r_["nl"]], [r_["nl"]])
            yield
            bk, rbk = rotbank()
            pe(lambda e, bk=bk: e.matmul(bk[:, 0:4], lhsT=tri, rhs=nl[:, :], start=True, stop=True),
               [r_cst, r_["nl"]], [rbk])
            pe(lambda e, bk=bk: e.matmul(bk[:, 4:8], lhsT=ones128, rhs=nl[:, :], start=True, stop=True),
               [r_cst, r_["nl"]], [rbk])
            dve(lambda e, bk=bk: e.scalar_tensor_tensor(out=gt[:, :], in0=bk[:, 0:4], scalar=-0.5 * math.log(128.0),
                                                 in1=ifs[:, 0:4], op0=ALU.add, op1=ALU.add),
                [rbk, r_["ifs"]], [r_["gt"]])
            act(lambda e: e.activation(out=g4[:, :], in_=gt[:, :], func=AF.Exp), [r_["gt"]], [r_["g4"]])
            act(lambda e, bk=bk: e.activation(out=thrc[:, :], in_=bk[:, 0:4], func=AF.Exp), [rbk], [rthr])
            act(lambda e, bk=bk: e.activation(out=decc[:, :], in_=bk[:, 4:8], func=AF.Exp, scale=-1.0), [rbk], [rdec])
            yield
            for half in range(2):
                bank, rb = rotbank()
                proj_fm(bank, rb, 1024 + half * 512, 4, hTc, rhT)
                act(lambda e, bank=bank, half=half: e.activation(
                    out=qkraw[:, half * 4:half * 4 + 4, 3:131], in_=bank[:, :].rearrange("p (j t) -> p j t", t=128),
                    func=AF.Copy), [rb], [r_["qkraw"]])
                yield
            for h in range(4):
                dve(lambda e, h=h: e.tensor_scalar(out=vtc[:, h, :], in0=vaug[:, h, :], scalar1=g4[:, h:h + 1],
                                                   scalar2=None, op0=ALU.mult), [r_["vaug"], r_["g4"]], [rvt])
                dve(lambda e, h=h: e.tensor_scalar(out=vhc[:, h, :], in0=vaug[:, h, :], scalar1=g4[:, h:h + 1],
                                                   scalar2=decc[:, h:h + 1], op0=ALU.mult, op1=ALU.mult),
                    [r_["vaug"], r_["g4"], rdec], [rvh])
            yield
            for half in range(2):
                bank, rb = rotbank()
                for jj in range(4):
                    j = half * 4 + jj
                    for tap in range(4):
                        pe(lambda e, bank=bank, j=j, jj=jj, tap=tap: e.matmul(
                            bank[:, jj * 128:(jj + 1) * 128], lhsT=dgq[:, j * 4 + tap, :],
                            rhs=qkraw[:, j, tap:tap + 128], start=(tap == 0), stop=False),
                           [r_dgq, r_["qkraw"]], [rb])
                    pe(lambda e, bank=bank, j=j, jj=jj: e.matmul(
                        bank[:, jj * 128:(jj + 1) * 128], lhsT=browb[0:1, j * 128:(j + 1) * 128], rhs=onesb[0:1, :],
                        start=False, stop=True), [r_["browb"], r_["onesb"]], [rb])
                sigmoid_chain(bank[:, :], rb, tq[:, :], r_["tq"])
                dve(lambda e, bank=bank, half=half: e.tensor_tensor(
                    out=qkTc[:, half * 4:half * 4 + 4, :], in0=bank[:, :].rearrange("p (j t) -> p j t", t=128),
                    in1=tq[:, :].rearrange("p (j t) -> p j t", t=128), op=ALU.mult), [rb, r_["tq"]], [rqkT])
                yield
            pool(lambda e: e.tensor_copy(out=qkraw[:, :, 0:3], in_=qkraw[:, :, 128:131]), [r_["qkraw"]], [r_["qkraw"]])
            bk, rbk = rotbank()
            proj_tm(bk, rbk, 2560, 512, hTc, rhT)
            sigmoid_chain(bk[:, :], rbk, soc[:, :], rso)
            yield
            for h in range(4):
                pe(lambda e, h=h: e.transpose(out=T0[:, h, :], in_=qkTc[:, 4 + h, :], identity=identb[:, :]),
                   [rqkT, r_identb], [rT0])
            act(lambda e: e.activation(out=ktmc[:, :].rearrange("p (h d) -> p h d", d=128), in_=T0[:, 0:4, :],
                                       func=AF.Copy), [rT0], [rktm])
            yield
            bk, rbk = rotbank()
            for h in range(4):
                pe(lambda e, h=h, bk=bk: e.matmul(bk[:, h * 128:(h + 1) * 128], lhsT=qkTc[:, 4 + h, :], rhs=qkTc[:, h, :],
                                           start=True, stop=True), [rqkT], [rbk])
            dve(lambda e, bk=bk: e.tensor_tensor(out=PTmc[:, :, :], in0=bk[:, :].rearrange("p (h t) -> p h t", t=128),
                                          in1=mask4.rearrange("p (h t) -> p h t", t=128), op=ALU.mult),
                [rbk, r_cst], [rPTm])
            yield
            bk, rbk = rotbank()
            proj_tm(bk, rbk, 3072, 512, hTc, rhT)
            sigmoid_chain(bk[:, :], rbk, tq[:, :], r_["tq"])
            dve(lambda e, bk=bk: e.tensor_tensor(out=mzc[:, :], in0=bk[:, :], in1=tq[:, :], op=ALU.mult),
                [rbk, r_["tq"]], [rmz])
            dve(lambda e: e.tensor_tensor(out=mzc[:, :], in0=mzc[:, :], in1=prm[:, P_MLG:P_MLG + 512], op=ALU.mult),
                [rmz, r_prm], [rmz])
            yield

        def mlsB(t):
            c = t % 2
            vtc, vhc, soc, mzc, thrc, decc = vt[c], vh[c], so[c], mz[c], thr[c], dec[c]
            qkTc, ktmc, PTmc = qkT[c], ktm[c], PTm[c]
            rvt, rvh, rso, rmz, rthr, rdec = (r_["vt%d" % c], r_["vh%d" % c], r_["so%d" % c], r_["mz%d" % c],
                                               r_["thr%d" % c], r_["dec%d" % c])
            rqkT, rktm, rPTm = r_["qkT%d" % c], r_["ktm%d" % c], r_["PTm%d" % c]
            for g in range(2):
                ob, ro = rotbank()
                for hh in range(2):
                    h = g * 2 + hh
                    c0 = hh * 129
                    pe(lambda e, c0=c0, h=h, ob=ob: e.matmul(ob[:, c0:c0 + 129], lhsT=PTmc[:, h, :], rhs=vtc[:, h, :],
                                                      start=True, stop=False), [rPTm, rvt], [ro])
                    pe(lambda e, c0=c0, h=h, ob=ob: e.matmul(ob[:, c0:c0 + 129], lhsT=qkTc[:, h, :], rhs=Cb[:, h, :],
                                                      start=False, stop=True), [rqkT, r_["Cb"]], [ro])
                act(lambda e, g=g, ob=ob: e.activation(
                    out=den[:, g * 2:g * 2 + 2], in_=ob[:, 0:258].rearrange("p (h d) -> p h d", d=129)[:, :, 128],
                    func=AF.Abs), [ro], [r_["den"]])
                dve(lambda e, g=g: e.tensor_tensor(out=den[:, g * 2:g * 2 + 2], in0=den[:, g * 2:g * 2 + 2],
                                                   in1=thrc[:, g * 2:g * 2 + 2], op=ALU.max),
                    [r_["den"], rthr], [r_["den"]])
                dve(lambda e, g=g: e.reciprocal(out=rden[:, g * 2:g * 2 + 2], in_=den[:, g * 2:g * 2 + 2]),
                    [r_["den"]], [r_["rden"]])
                for hh in range(2):
                    h = g * 2 + hh
                    c0 = hh * 129
                    dve(lambda e, c0=c0, h=h, ob=ob: e.scalar_tensor_tensor(
                        out=hm[:, h * 128:(h + 1) * 128], in0=ob[:, c0:c0 + 128], scalar=rden[:, h:h + 1],
                        in1=soc[:, h * 128:(h + 1) * 128], op0=ALU.mult, op1=ALU.mult),
                        [ro, r_["rden"], rso], [r_["hm"]])
                yield
            for g in range(2):
                ob, ro = rotbank()
                for hh in range(2):
                    h = g * 2 + hh
                    c0 = hh * 129
                    pe(lambda e, c0=c0, h=h, ob=ob: e.matmul(ob[:, c0:c0 + 129], lhsT=ktmc[:, h * 128:(h + 1) * 128],
                                                      rhs=vhc[:, h, :], start=True, stop=True), [rktm, rvh], [ro])
                for hh in range(2):
                    h = g * 2 + hh
                    c0 = hh * 129
                    dve(lambda e, c0=c0, h=h, ob=ob: e.scalar_tensor_tensor(
                        out=Cst[:, h, :], in0=Cst[:, h, :], scalar=decc[:, h:h + 1], in1=ob[:, c0:c0 + 129],
                        op0=ALU.mult, op1=ALU.add), [ro, rdec, r_["Cst"]], [r_["Cst"]])
                yield
            act(lambda e: e.activation(out=Cb[:, :, :], in_=Cst[:, :, :], func=AF.Copy), [r_["Cst"]], [r_["Cb"]])
            dve(lambda e: e.tensor_tensor(out=sq2[:, :], in0=hm[:, :], in1=hm[:, :], op=ALU.mult), [r_["hm"]], [r_["sq2"]])
            dve(lambda e: e.tensor_reduce(out=st4[:, :], in_=sq2[:, :].rearrange("p (g d) -> p g d", d=128),
                                          axis=AX.X, op=ALU.add), [r_["sq2"]], [r_["st4"]])
            rstd_chain(st4[:, :], r_["st4"], 1.0 / 128)
            yield
            for h in range(4):
                dve(lambda e, h=h: e.scalar_tensor_tensor(
                    out=mixed[c][:, 256 + h * 128:256 + (h + 1) * 128], in0=hm[:, h * 128:(h + 1) * 128],
                    scalar=st4[:, h:h + 1], in1=mzc[:, h * 128:(h + 1) * 128], op0=ALU.mult, op1=ALU.mult),
                    [r_["hm"], r_["st4"], rmz], [r_["mixed%d" % c]])
            yield

        def cnvA(t):
            c = t % 2
            hTc, rhT = hT[c], r_["hT%d" % c]
            uTc, ruT = uT[c], r_["uT%d" % c]
            czc, rcz = czs[c], r_["czs%d" % c]
            pool(lambda e: e.tensor_copy(out=uTc[:, :, 0:30], in_=uT[1 - c][:, :, 128:158]),
                 [r_["uT%d" % (1 - c)]], [ruT])
            bank, rb = rotbank()
            proj_fm(bank, rb, 3592, 4, hTc, rhT)
            sigmoid_chain(bank[:, 256:512], rb, tcz[:, :], r_["tcz"])
            dve(lambda e, bank=bank: e.tensor_tensor(out=uTc[:, :, 30:158],
                                                     in0=bank[:, 0:256].rearrange("p (a b) -> p a b", b=128),
                                                     in1=tcz[:, :].rearrange("p (a b) -> p a b", b=128), op=ALU.mult),
                [rb, r_["tcz"]], [ruT])
            yield
            bank, rb = rotbank()
            proj_tm(bank, rb, 4104, 256, hTc, rhT)
            sigmoid_chain(bank[:, 0:256], rb, tcz2[:, :], r_["tcz"])
            dve(lambda e, bank=bank: e.tensor_tensor(out=czc[:, :], in0=bank[:, 0:256], in1=tcz2[:, :], op=ALU.mult),
                [rb, r_["tcz"]], [rcz])
            yield

        def cnvB(t):
            c = t % 2
            uTc, ruT = uT[c], r_["uT%d" % c]
            czc, rcz = czs[c], r_["czs%d" % c]
            for ct in range(2):
                for j in range(31):
                    pe(lambda e, ct=ct, j=j: e.matmul(
                        FB[0][:, ct * 128:(ct + 1) * 128], lhsT=uTc[:, ct, j:j + 128], rhs=dgc[:, ct * 31 + j, :],
                        start=(j == 0), stop=(j == 30)), [ruT, r_dgc], [rF[0]])
                    if j % 8 == 7:
                        yield
            dve(lambda e: e.tensor_tensor(out=uc[:, :], in0=FB[0][:, 0:256], in1=prm[:, P_DWB:P_DWB + 256],
                                          op=ALU.add), [rF[0], r_prm], [r_["uc"]])
            dve(lambda e: e.bn_stats(out=bst[:, :], in_=uc[:, :]), [r_["uc"]], [r_["bst"]])
            dve(lambda e: e.bn_aggr(out=mv[:, :], in_=bst[:, :]), [r_["bst"]], [r_["mv"]])
            act(lambda e: e.activation(out=rstd1[:, :], in_=mv[:, 1:2], func=AF.Ln, bias=EPS), [r_["mv"]], [r_["rstd1"]])
            act(lambda e: e.activation(out=rstd1[:, :], in_=rstd1[:, :], func=AF.Exp, scale=-0.5),
                [r_["rstd1"]], [r_["rstd1"]])
            yield
            dve(lambda e: e.tensor_scalar(out=yln[:, :], in0=uc[:, :], scalar1=mv[:, 0:1], scalar2=rstd1[:, 0:1],
                                          op0=ALU.subtract, op1=ALU.mult), [r_["uc"], r_["mv"], r_["rstd1"]], [r_["yln"]])
            dve(lambda e: e.tensor_tensor(out=yln[:, :], in0=yln[:, :], in1=prm[:, P_LNG:P_LNG + 256], op=ALU.mult),
                [r_["yln"], r_prm], [r_["yln"]])
            dve(lambda e: e.tensor_tensor(out=yln[:, :], in0=yln[:, :], in1=prm[:, P_LNB:P_LNB + 256], op=ALU.add),
                [r_["yln"], r_prm], [r_["yln"]])
            sigmoid_chain(yln[:, :], r_["yln"], tyl2[:, :], r_["tyl2"])
            yield
            dve(lambda e: e.tensor_tensor(out=tyl2[:, :], in0=tyl2[:, :], in1=yln[:, :], op=ALU.mult),
                [r_["tyl2"], r_["yln"]], [r_["tyl2"]])
            dve(lambda e: e.tensor_tensor(out=mixed[c][:, 768:1024], in0=tyl2[:, :], in1=czc[:, :], op=ALU.mult),
                [r_["tyl2"], rcz], [r_["mixed%d" % c]])
            yield

        def tail(t, l=l, dst_d=dst_d):
            b = t % 4
            c = t % 2
            X, rX = xt[b], r_x[b]
            for k in range(8):
                pe(lambda e, k=k: e.transpose(out=T0[:, k, :], in_=mixed[c][:, k * 128:(k + 1) * 128], identity=identb[:, :]),
                   [r_["mixed%d" % c], r_identb], [rT0])
            act(lambda e: e.activation(out=mT[:, 0:4, :], in_=T0[:, 0:4, :], func=AF.Copy), [rT0], [r_["mT"]])
            dve(lambda e: e.tensor_copy(out=mT[:, 4:8, :], in_=T0[:, 4:8, :]), [rT0], [r_["mT"]])
            yield
            for nb in range(2):
                bank, rb = rotbank()
                for k in range(8):
                    pe(lambda e, bank=bank, k=k, nb=nb: e.matmul(bank[:, :], lhsT=mT[:, k, :],
                                                                 rhs=w_out[:, k, nb * 512:(nb + 1) * 512],
                                                                 start=(k == 0), stop=(k == 7)),
                       [r_["mT"], r_wout], [rb])
                dve(lambda e, bank=bank, nb=nb: e.tensor_tensor(
                    out=X[:, nb * 512:(nb + 1) * 512], in0=bank[:, :], in1=X[:, nb * 512:(nb + 1) * 512], op=ALU.add),
                    [rb], [rX])
                yield
            wr = [r_mid[t]] if l < L - 1 else []
            sch.dma("pool", "d_o%d" % b,
                    lambda e: e.dma_start(out=dst_d[t * 128:(t + 1) * 128, :], in_=X[:, :]), [rX], wr)
            yield

        def drive(gens):
            gens = list(gens)
            while gens:
                for g in list(gens):
                    try:
                        next(g)
                    except StopIteration:
                        gens.remove(g)

        load(0)
        if NT > 1:
            load(1)
        drive([front(0)])
        for st in range(NT + 2):
            gens = []
            if 1 <= st <= NT:
                gens.append(cnvB(st - 1))
            if 2 <= st <= NT + 1:
                gens.append(tail(st - 2))
            if st < NT:
                gens += [cnvA(st), attA(st), mlsA(st)]
            if st + 1 < NT:
                gens.append(front(st + 1))
            if 1 <= st <= NT:
                gens += [mlsB(st - 1), attB(st - 1)]
            drive(gens)
            if st + 2 < NT:
                load(st + 2)
    final_waits = [(k, v) for k, v in sch.dma_cnt.items() if k.startswith("d_o")]

    with nc.Block() as block:
        ctxs = sch.emit(nc, block, final_waits)
    return nc


def _host_prep(inputs, L):
    f = np.float32
    prm = np.zeros((L, 128, NPRM), f)
    attb = np.zeros((L, 128, 20, 128), f)
    ql = np.arange(128)[None, :]
    kl = np.arange(128)[:, None]
    for l in range(L):
        prm[l, :, P_GIN:P_GIN + 8] = inputs["norm_g"][l].reshape(8, 128).T
        prm[l, :, P_QG:P_QG + 64] = inputs["att_q_g"][l][None, :]
        prm[l, :, P_KG:P_KG + 64] = inputs["att_k_g"][l][None, :]
        prm[l, :, P_MLG:P_MLG + 512] = inputs["ml_out_g"][l][None, :]
        prm[l, :, P_LNG:P_LNG + 256] = inputs["cv_ln_g"][l][None, :]
        prm[l, :, P_LNB:P_LNB + 256] = inputs["cv_ln_b"][l][None, :]
        prm[l, :, P_DWB:P_DWB + 256] = inputs["cv_dw_b"][l][None, :]
        prm[l, :, P_BIF:P_BIF + 4] = inputs["ml_b_i"][l][None, :]
        prm[l, :, P_BIF + 4:P_BIF + 8] = inputs["ml_b_f"][l][None, :]
        w = inputs["ml_qk_conv_w"][l]
        prm[l, :, P_QKCW:P_QKCW + 32] = w.reshape(4, 8, 128).transpose(2, 1, 0).reshape(128, 32)
        prm[l, :, P_QKCB:P_QKCB + 8] = inputs["ml_qk_conv_b"][l].reshape(8, 128).T
        w = inputs["cv_dw_w"][l]
        prm[l, :, P_DWW:P_DWW + 62] = w.reshape(31, 2, 128).transpose(2, 1, 0).reshape(128, 62)
        rb = inputs["att_rel_bias"][l]
        for j in range(5):
            rel = ql - kl + (8 - 2 * j) * 64
            idx = np.clip(rel, -128, 128) + 128
            dd = ql // 64 - kl // 64 + 8 - 2 * j
            valid = (dd >= 0) & (dd <= 8)
            for h in range(4):
                attb[l, :, h * 5 + j, :] = np.where(valid, rb[h][idx], f(NEGB))
    cst = np.zeros((128, 896), f)
    cst[:, 0:128] = np.eye(128, dtype=f)
    tri = (np.arange(128)[:, None] <= np.arange(128)[None, :]).astype(f)
    cst[:, 128:256] = tri
    cst[:, 256:768] = np.tile(tri, (1, 4))
    cst[:, 768:896] = 1.0
    rowp = np.ascontiguousarray(inputs["ml_qk_conv_b"][:L].reshape(L, 1, 1024), dtype=f)
    return prm, attb.reshape(L, 128, 2560), cst, rowp


def kernel(**inputs):
    inputs = {k: np.asarray(v) for k, v in inputs.items()}
    x = inputs["x"]
    B, S, _ = x.shape
    L = inputs["w_in"].shape[0]
    prm, attb, cst, rowp = _host_prep(inputs, L)
    nc = build_nc(S, L)
    w_in = np.ascontiguousarray(inputs["w_in"], dtype=np.float32)
    w_out = np.ascontiguousarray(inputs["w_out"], dtype=np.float32)
    in_maps = [{"x": np.ascontiguousarray(x[i]), "w_in": w_in, "w_out": w_out, "prm": prm, "attb": attb, "cst": cst,
                "rowp": rowp} for i in range(B)]
    res = run_bass_kernel_spmd(nc, in_maps, core_ids=list(range(B)))
    return np.stack([np.asarray(r["out"]) for r in res.results], axis=0).astype(np.float32)
```
